# Optimizing a Trainium2 kernel written in Bass

```python
import jax, jax.numpy as jnp
from jax import lax
import numpy as np

D_MODEL = 2048
BATCH = 4
SEQ = 4096
DEPTH = 4

N_MIXERS = 2
N_HEADS = 16
N_KV_HEADS = 4
HEAD_DIM = D_MODEL // N_HEADS
GROUP = N_HEADS // N_KV_HEADS
ROPE_DIM = HEAD_DIM // 4
ROPE_THETA = 500000.0
IDX_HEADS = 16
IDX_DIM = 64
IDX_ROPE_DIM = IDX_DIM // 4
TOPK_MAX = 256
Q_BLOCK = 128
Q_W = N_HEADS * HEAD_DIM
KV_W = N_KV_HEADS * HEAD_DIM
IDX_Q_W = IDX_HEADS * IDX_DIM
ATTN_PROJ = Q_W + 2 * KV_W + IDX_Q_W + IDX_DIM + IDX_HEADS
CONV_WIDTH = 3
D_FF = 4 * D_MODEL
EPS = 1e-6
N_ATTN_LAYERS = (DEPTH + 1) // 2
N_CONV_LAYERS = DEPTH // 2
MAX_POS_OFFSET = 1024

kernel_name = "hybrid_dsa_shortconv_sqrelu_trunk"


def _rmsnorm(x, g):
    xf = x.astype(jnp.float32)
    y = xf * lax.rsqrt(jnp.mean(xf * xf, axis=-1, keepdims=True) + EPS)
    return (y * g.astype(jnp.float32)).astype(x.dtype)


def _rope_tables(positions, rot_dim, dtype):
    inv = ROPE_THETA ** (-jnp.arange(0, rot_dim, 2, dtype=jnp.float32) / rot_dim)
    ang = positions.astype(jnp.float32)[..., None] * inv
    return jnp.cos(ang)[:, :, None, :].astype(dtype), jnp.sin(ang)[:, :, None, :].astype(dtype)


def _partial_rope(x, cos, sin):
    half = cos.shape[-1]
    x1 = x[..., :half]
    x2 = x[..., half:2 * half]
    return jnp.concatenate([x1 * cos - x2 * sin, x2 * cos + x1 * sin, x[..., 2 * half:]], axis=-1)


def _dsa_mixer(h, w_in, q_g, k_g, w_out, positions):
    B, S, _ = h.shape
    proj = h @ w_in
    cuts = [Q_W, Q_W + KV_W, Q_W + 2 * KV_W, Q_W + 2 * KV_W + IDX_Q_W,
            Q_W + 2 * KV_W + IDX_Q_W + IDX_DIM]
    q, k, v, qi, ki, wi = jnp.split(proj, cuts, axis=-1)
    q = _rmsnorm(q.reshape(B, S, N_HEADS, HEAD_DIM), q_g)
    k = _rmsnorm(k.reshape(B, S, N_KV_HEADS, HEAD_DIM), k_g)
    v = v.reshape(B, S, N_KV_HEADS, HEAD_DIM)
    cos, sin = _rope_tables(positions, ROPE_DIM, h.dtype)
    q = _partial_rope(q, cos, sin)
    k = _partial_rope(k, cos, sin)
    ci, si = _rope_tables(positions, IDX_ROPE_DIM, h.dtype)
    qi = _partial_rope(qi.reshape(B, S, IDX_HEADS, IDX_DIM), ci, si)
    ki = _partial_rope(ki.reshape(B, S, 1, IDX_DIM), ci, si)[:, :, 0]
    wi = wi * (IDX_HEADS ** -0.5 * IDX_DIM ** -0.5)

    top_k = min(TOPK_MAX, S // 4)
    qb = min(Q_BLOCK, S)
    nb = S // qb
    scale = HEAD_DIM ** -0.5
    key_pos = jnp.arange(S)

    def to_blocks(a):
        return a.reshape((B, nb, qb) + a.shape[2:]).swapaxes(0, 1)

    def block(args):
        q_blk, qi_blk, wi_blk, b_idx = args
        t = b_idx * qb + jnp.arange(qb)
        rel = jax.nn.relu(jnp.einsum('bqhd,bsd->bqhs', qi_blk, ki)).astype(jnp.float32)
        score = jnp.einsum('bqhs,bqh->bqs', rel, wi_blk.astype(jnp.float32))
        causal = key_pos[None, :] <= t[:, None]
        score = jnp.where(causal[None], score, -jnp.inf)
        _, idx = lax.top_k(score, top_k)
        valid = idx <= t[None, :, None]
        k_sel = jax.vmap(lambda kb, ib: kb[ib])(k, idx)
        v_sel = jax.vmap(lambda vb, ib: vb[ib])(v, idx)
        qg = q_blk.reshape(B, qb, N_KV_HEADS, GROUP, HEAD_DIM)
        s = jnp.einsum('bqngd,bqknd->bqngk', qg, k_sel).astype(jnp.float32) * scale
        s = jnp.where(valid[:, :, None, None, :], s, -jnp.inf)
        p = jax.nn.softmax(s, axis=-1).astype(v.dtype)
        o = jnp.einsum('bqngk,bqknd->bqngd', p, v_sel)
        return o.reshape(B, qb, Q_W)

    out = lax.map(block, (to_blocks(q), to_blocks(qi), to_blocks(wi), jnp.arange(nb)))
    out = out.swapaxes(0, 1).reshape(B, S, Q_W)
    return out @ w_out


def _short_conv_mixer(h, w_in, conv_w, w_out):
    S = h.shape[1]
    b_gate, c_gate, u = jnp.split(h @ w_in, 3, axis=-1)
    z = c_gate * u
    zp = jnp.pad(z, ((0, 0), (CONV_WIDTH - 1, 0), (0, 0)))
    y = sum(zp[:, j:j + S] * conv_w[j] for j in range(CONV_WIDTH))
    return (b_gate * y) @ w_out


def _sqrelu_mlp(h, w1, w2):
    a = jax.nn.relu(h @ w1)
    return (a * a) @ w2


def setup_inputs(seed: int = 0) -> dict:
    key = jax.random.key(seed)
    ks = jax.random.split(key, 16)
    f32 = jnp.float32

    def nrm(k, shape, fan_in):
        return jax.random.normal(k, shape, f32) * (fan_in ** -0.5)

    def gain(k, shape):
        return 1.0 + 0.02 * jax.random.normal(k, shape, f32)

    x = jax.random.normal(ks[0], (BATCH, SEQ, D_MODEL), f32)
    offs = jax.random.randint(ks[1], (BATCH, 1), 0, MAX_POS_OFFSET, dtype=jnp.int32)
    positions = offs + jnp.arange(SEQ, dtype=jnp.int32)[None, :]
    return {
        "x": x,
        "positions": positions,
        "attn_norm_g": gain(ks[2], (N_ATTN_LAYERS, D_MODEL)),
        "attn_w_in": nrm(ks[3], (N_ATTN_LAYERS, D_MODEL, ATTN_PROJ), D_MODEL),
        "attn_q_norm_g": gain(ks[4], (N_ATTN_LAYERS, HEAD_DIM)),
        "attn_k_norm_g": gain(ks[5], (N_ATTN_LAYERS, HEAD_DIM)),
        "attn_w_out": nrm(ks[6], (N_ATTN_LAYERS, Q_W, D_MODEL), Q_W),
        "conv_norm_g": gain(ks[7], (N_CONV_LAYERS, D_MODEL)),
        "conv_w_in": nrm(ks[8], (N_CONV_LAYERS, D_MODEL, 3 * D_MODEL), D_MODEL),
        "conv_w": nrm(ks[9], (N_CONV_LAYERS, CONV_WIDTH, D_MODEL), CONV_WIDTH),
        "conv_w_out": nrm(ks[10], (N_CONV_LAYERS, D_MODEL, D_MODEL), D_MODEL),
        "mlp_norm_g": gain(ks[11], (DEPTH, D_MODEL)),
        "mlp_w1": nrm(ks[12], (DEPTH, D_MODEL, D_FF), D_MODEL),
        "mlp_w2": nrm(ks[13], (DEPTH, D_FF, D_MODEL), D_FF),
    }


def reference(x, positions, attn_norm_g, attn_w_in, attn_q_norm_g, attn_k_norm_g, attn_w_out,
              conv_norm_g, conv_w_in, conv_w, conv_w_out, mlp_norm_g, mlp_w1, mlp_w2):
    for i in range(DEPTH):
        j = i // N_MIXERS
        if i % N_MIXERS == 0:
            x = x + _dsa_mixer(_rmsnorm(x, attn_norm_g[j]), attn_w_in[j], attn_q_norm_g[j],
                               attn_k_norm_g[j], attn_w_out[j], positions)
        else:
            x = x + _short_conv_mixer(_rmsnorm(x, conv_norm_g[j]), conv_w_in[j], conv_w[j],
                                      conv_w_out[j])
        x = x + _sqrelu_mlp(_rmsnorm(x, mlp_norm_g[i]), mlp_w1[i], mlp_w2[i])
    return x
```

```python
import bisect
import contextlib
import numpy as np
import concourse.bass as bass
import concourse.mybir as mybir
from concourse.bass_utils import run_bass_kernel_spmd

F32 = mybir.dt.float32
BF16 = mybir.dt.bfloat16
I32 = mybir.dt.int32
AF = mybir.ActivationFunctionType
ALU = mybir.AluOpType
AX = mybir.AxisListType

D = 2048
KC = 16
TOK = 2048
BLK = 512
NB = TOK // BLK
DFF = 8192
NFC = DFF // 128
EPS = 1e-6
BIG = 30000.0
TOPK = 256
NBIS = 12
G = 4
ROPE_THETA = 500000.0


class Buf:
    def __init__(self, ap, const=False):
        self.ap = ap
        self.w = {}
        self.r = {}
        self.const = const

    def __getitem__(self, k):
        return self.ap[k]


def _merge(d, t):
    if t[0] == 'e':
        key = ('e', id(t[1]))
        if key not in d or d[key][2] < t[2]:
            d[key] = t
    else:
        key = ('d', t[1].num)
        if key not in d or d[key][2] < t[2]:
            d[key] = t


class Eng:
    def __init__(self, K, e, name, is_pe=False):
        self.K = K
        self.e = e
        self.sem = K.newsem("s_" + name)
        self.val = 0
        self.seq = 0
        self.last = None
        self.sigseq = []
        self.sigval = []
        self.seen = {}
        self.is_pe = is_pe
        self.eager = False

    def value_for(self, seq):
        i = bisect.bisect_left(self.sigseq, seq)
        if i < len(self.sigseq):
            return self.sigval[i]
        assert self.last is not None and self.seq >= seq
        self.last.then_inc(self.sem, 1)
        self.val += 1
        self.sigseq.append(self.seq)
        self.sigval.append(self.val)
        return self.val

    def wait(self, tks):
        for t in tks:
            if t[0] == 'e':
                src = t[1]
                if src is self and self.is_pe:
                    continue
                sem = src.sem
                v = src.value_for(t[2])
            else:
                sem, v = t[1], t[2]
            if self.seen.get(sem.num, 0) >= v:
                continue
            self.e.wait_ge(sem, v)
            self.seen[sem.num] = v

    def deps(self, reads, writes):
        d = []
        for b in reads:
            d.extend(b.w.values())
        for b in writes:
            d.extend(b.w.values())
            d.extend(b.r.values())
        return d

    def op(self, fn, r=(), w=()):
        self.wait(self.deps(r, w))
        ins = fn()
        self.seq += 1
        self.last = ins
        if self.eager:
            ins.then_inc(self.sem, 1)
            self.val += 1
            self.sigseq.append(self.seq)
            self.sigval.append(self.val)
        t = ('e', self, self.seq)
        for b in r:
            if not b.const:
                _merge(b.r, t)
        for b in w:
            b.w = {}
            b.r = {}
            _merge(b.w, t)
        return t


class DmaQ:
    def __init__(self, K, eng, name, nsem):
        self.eng = eng
        self.sems = [K.newsem("d_%s%d" % (name, i)) for i in range(nsem)]
        self.vals = [0] * nsem
        self.i = 0

    def dma(self, out, in_, r=(), w=()):
        E = self.eng
        k = self.i % len(self.sems)
        self.i += 1
        d = E.deps(r, w)
        if self.vals[k] > 0:
            d.append(('d', self.sems[k], self.vals[k]))
        E.wait(d)
        ins = E.e.dma_start(out=out, in_=in_)
        ins.then_inc(self.sems[k], 16)
        self.vals[k] += 16
        t = ('d', self.sems[k], self.vals[k])
        for b in r:
            if not b.const:
                _merge(b.r, t)
        for b in w:
            b.w = {}
            b.r = {}
            _merge(b.w, t)
        return t


class Kern:
    def __init__(self):
        self.nc = bass.Bass("TRN2", target_bir_lowering=False)
        self.es = contextlib.ExitStack()
        self.phase = None
        nc = self.nc
        self.pe = Eng(self, nc.tensor, "pe", is_pe=True)
        self.act = Eng(self, nc.scalar, "act")
        self.dve = Eng(self, nc.vector, "dve")
        self.pool = Eng(self, nc.gpsimd, "pool")
        self.sp = Eng(self, nc.sync, "sp")
        self.engs = [self.pe, self.act, self.dve, self.pool, self.sp]
        self.act.eager = True
        self.dve.eager = True
        self.qs = DmaQ(self, self.sp, "sp", 10)
        self.qg = DmaQ(self, self.pool, "pl", 10)
        self.qs2 = DmaQ(self, self.sp, "so", 4)
        self.n_uid = 0
        self.ps = []
        for i in range(8):
            t = self.es.enter_context(nc.psum_tensor("ps%d" % i, [128, 512], F32))
            self.ps.append(Buf(t))

    def newsem(self, name):
        return self.es.enter_context(self.nc.semaphore(name))

    def uid(self, p):
        self.n_uid += 1
        return "%s_%d" % (p, self.n_uid)

    def begin_phase(self):
        self.phase = contextlib.ExitStack()

    def end_phase(self):
        self.barrier()
        self.phase.close()
        self.phase = None

    def sb(self, name, shape, dtype, const=False, perm=False):
        st = self.es if perm else self.phase
        t = st.enter_context(self.nc.sbuf_tensor(self.uid(name), list(shape), dtype))
        return Buf(t, const=const)

    def dram(self, name, shape, dtype, kind=None):
        if kind is None:
            t = self.nc.dram_tensor(name, list(shape), dtype)
        else:
            t = self.nc.dram_tensor(name, list(shape), dtype, kind=kind)
        return Buf(t)

    def barrier(self):
        tks = []
        for E in self.engs:
            if E.seq > 0:
                tks.append(('e', E, E.seq))
        for q in (self.qs, self.qg, self.qs2):
            for s, v in zip(q.sems, q.vals):
                if v > 0:
                    tks.append(('d', s, v))
        for E in self.engs:
            E.wait([t for t in tks if not (t[0] == 'e' and t[1] is E)])

    def V(self, fn, r=(), w=()):
        return self.dve.op(fn, r, w)

    def A(self, fn, r=(), w=()):
        return self.act.op(fn, r, w)

    def T(self, fn, r=(), w=()):
        return self.pe.op(fn, r, w)


class Ring:
    def __init__(self, K, n, elems=4096):
        self.K = K
        self.slots = [K.sb("ring", [128, elems], BF16) for _ in range(n)]
        self.i = 0

    def get(self, src_ap, shape, srcbuf):
        K = self.K
        s = self.slots[self.i % len(self.slots)]
        self.i += 1
        n = int(np.prod(shape[1:]))
        v = s.ap[:, 0:n]
        if len(shape) == 3:
            v = v.rearrange("p (a b) -> p a b", a=shape[1])
        K.qg.dma(out=v, in_=src_ap, r=[srcbuf], w=[s])
        return s, v


def bf16v(psbuf):
    return psbuf.ap[:, :].bitcast(BF16)


class Ctx:
    pass


def setup_consts(K, C, io):
    nc = K.nc
    C.ident = K.sb("ident", [128, 128], BF16, const=True, perm=True)
    K.qg.dma(out=C.ident.ap[:, :], in_=io.ident.ap[:, :], r=[io.ident], w=[C.ident])
    C.eps = K.sb("eps", [128, 1], F32, const=True, perm=True)
    K.V(lambda: nc.vector.memset(C.eps.ap[:, :], EPS), w=[C.eps])
    C.mcol = K.sb("mcol", [128, 1], F32, const=True, perm=True)
    K.qs.dma(out=C.mcol.ap[:, :], in_=io.halo_m.ap[:, :], r=[io.halo_m], w=[C.mcol])
    C.cw = K.sb("cw", [128, 96], F32, const=True, perm=True)
    K.qs.dma(out=C.cw.ap[:, :], in_=io.convw.ap[:, :], r=[io.convw], w=[C.cw])


def norm_block(K, C, xsrc, row0, ntok, g_b, hn, xs, hnT, hnT_parts, st, tb):
    nc = K.nc
    nsub = (ntok + 127) // 128
    xs_l = xs if isinstance(xs, list) else [xs]
    hn_l = hn if isinstance(hn, list) else [hn]
    st_l = st if isinstance(st, list) else [st]
    for ts in range(nsub):
        xs, hn, st = xs_l[ts % len(xs_l)], hn_l[ts % len(hn_l)], st_l[ts % len(st_l)]
        rows = min(128, ntok - ts * 128)
        if rows < 128:
            K.V(lambda: nc.vector.memset(xs.ap[:, :], 0.0), w=[xs])
        K.qs.dma(out=xs.ap[0:rows, :], in_=xsrc.ap[row0 + ts * 128: row0 + ts * 128 + rows, :],
                 r=[xsrc], w=[xs])
        K.A(lambda: nc.scalar.activation(out=hn.ap[:, :], in_=xs.ap[:, :], func=AF.Square,
                                         accum_out=st.ap[:, 0:1]), r=[xs], w=[hn, st])
        K.A(lambda: nc.scalar.activation(out=st.ap[:, 1:2], in_=st.ap[:, 0:1], func=AF.Sqrt,
                                         scale=1.0 / D, bias=C.eps.ap[:, 0:1]), r=[st, C.eps], w=[st])
        K.V(lambda: nc.vector.reciprocal(out=st.ap[:, 2:3], in_=st.ap[:, 1:2]), r=[st], w=[st])
        K.V(lambda: nc.vector.scalar_tensor_tensor(out=hn.ap[:, :], in0=xs.ap[:, :], scalar=st.ap[:, 2:3],
                                                   in1=g_b.ap[:, :], op0=ALU.mult, op1=ALU.mult),
            r=[xs, st, g_b], w=[hn])
        for half in range(2):
            pb = tb[half]
            pv = bf16v(pb)
            for k in range(8):
                kc = half * 8 + k
                K.T(lambda: nc.tensor.transpose(pv[:, k * 128:(k + 1) * 128], hn.ap[:, kc * 128:(kc + 1) * 128],
                                                C.ident.ap[:, :]), r=[hn, C.ident], w=[pb])
            dst = hnT.ap[:, half * 8:(half + 1) * 8, ts * 128:(ts + 1) * 128]
            src = pv.rearrange("p (k t) -> p k t", k=8)
            part = hnT_parts[ts * 2 + half]
            if half == 0:
                K.A(lambda: nc.scalar.copy(out=dst, in_=src), r=[pb], w=[part])
            else:
                K.V(lambda: nc.vector.tensor_copy(out=dst, in_=src), r=[pb], w=[part])


def load_gb(K, g_b, gsrc, row):
    K.qs.dma(out=g_b.ap[:, :], in_=gsrc.ap[row:row + 1, :].partition_broadcast(128), r=[gsrc], w=[g_b])


def mlp_half(K, C, io, li, xin, xout, after_store=None):
    nc = K.nc
    MB = 1024
    NS = MB // 128
    K.begin_phase()
    ring = Ring(K, 10, 2048)
    g_b = K.sb("g_b", [128, D], F32)
    hn = [K.sb("hn", [128, D], BF16) for _ in range(2)]
    xs = [K.sb("xs", [128, D], F32) for _ in range(2)]
    st = [K.sb("st", [128, 4], F32) for _ in range(2)]
    hnT = K.sb("hnT", [128, KC, MB], BF16)
    parts = [Buf(hnT.ap) for _ in range(2 * NS)]
    xacc = K.sb("xacc", [128, NS, D], F32)
    xparts = [[Buf(xacc.ap) for _ in range(4)] for _ in range(NS)]
    xflat = [b for row in xparts for b in row]
    a2T = K.sb("a2T", [128, 2 * G, MB], BF16)
    a2p = [Buf(a2T.ap) for _ in range(2 * G)]
    rt = [K.sb("rt", [128, 512], F32) for _ in range(2)]
    load_gb(K, g_b, io.mlp_g, li)
    nmm = 0
    nrt = 0
    for blk in range(TOK // MB):
        r0 = blk * MB
        for sb_ in range(MB // 512):
            norm_block(K, C, xin, r0 + sb_ * 512, 512, g_b, hn, xs,
                       Buf(hnT.ap[:, :, sb_ * 512:(sb_ + 1) * 512]), parts[sb_ * 8:(sb_ + 1) * 8], st, [K.ps[6], K.ps[7]])
        K.qs.dma(out=xacc.ap[:, :, :], in_=xin.ap[r0:r0 + MB, :].rearrange("(t p) d -> p t d", p=128),
                 r=[xin], w=xflat)
        for g in range(NFC // G):
            w2v = []
            for fi in range(G):
                fc = g * G + fi
                sidx = (g % 2) * G + fi
                slot = a2p[sidx]
                ws, wv = ring.get(io.w1.ap[li, fc], [128, KC, 128], io.w1)
                for th in range(MB // 512):
                    pb = K.ps[nmm % 3]
                    nmm += 1
                    for kc in range(KC):
                        K.T(lambda: nc.tensor.matmul(pb.ap[:, :], wv[:, kc, :], hnT.ap[:, kc, th * 512:(th + 1) * 512],
                                                     start=(kc == 0), stop=(kc == KC - 1)),
                            r=[ws] + parts, w=[pb])
                    rb = rt[nrt % 2]
                    nrt += 1
                    K.A(lambda: nc.scalar.activation(out=rb.ap[:, :], in_=pb.ap[:, :], func=AF.Relu), r=[pb], w=[rb])
                    K.V(lambda: nc.vector.tensor_tensor(out=a2T.ap[:, sidx, th * 512:(th + 1) * 512], in0=rb.ap[:, :],
                                                        in1=rb.ap[:, :], op=ALU.mult), r=[rb], w=[slot])
            for fi in range(G):
                fc = g * G + fi
                w2v.append(ring.get(io.w2.ap[li, fc], [128, D], io.w2))
            for ts in range(NS):
                for dc in range(4):
                    pb = K.ps[3 + (ts * 4 + dc) % 3]
                    for fi in range(G):
                        sidx = (g % 2) * G + fi
                        K.T(lambda: nc.tensor.matmul(pb.ap[:, :], a2T.ap[:, sidx, ts * 128:(ts + 1) * 128],
                                                     w2v[fi][1][:, dc * 512:(dc + 1) * 512],
                                                     start=(fi == 0), stop=(fi == G - 1)),
                            r=[a2p[sidx], w2v[fi][0]], w=[pb])
                    xp = xparts[ts][dc]
                    K.V(lambda: nc.vector.tensor_tensor(out=xacc.ap[:, ts, dc * 512:(dc + 1) * 512],
                                                        in0=pb.ap[:, :],
                                                        in1=xacc.ap[:, ts, dc * 512:(dc + 1) * 512], op=ALU.add),
                        r=[pb], w=[xp])
        K.qs2.dma(out=xout.ap[r0:r0 + MB, :].rearrange("(t p) d -> p t d", p=128), in_=xacc.ap[:, :, :],
                  r=xflat, w=[xout])
        if after_store is not None:
            for b5 in range(MB // 512):
                after_store(blk * (MB // 512) + b5)
    K.end_phase()


def conv_half(K, C, io, cj, xin, xhalo, xout):
    nc = K.nc
    K.begin_phase()
    ring = Ring(K, 8, 4096)
    g_b = K.sb("g_b", [128, D], F32)
    hn = [K.sb("hn", [128, D], BF16) for _ in range(2)]
    xs = [K.sb("xs", [128, D], F32) for _ in range(2)]
    st = [K.sb("st", [128, 4], F32) for _ in range(2)]
    hnT = K.sb("hnT", [128, KC, BLK], BF16)
    parts = [Buf(hnT.ap) for _ in range(8)]
    hnTh = K.sb("hnTh", [128, KC, 128], BF16)
    hparts = [Buf(hnTh.ap) for _ in range(8)]
    xacc = K.sb("xacc", [128, 4, D], F32)
    xparts = [[Buf(xacc.ap) for _ in range(8)] for _ in range(4)]
    xflat = [b for row in xparts for b in row]
    gyT = K.sb("gyT", [128, KC, BLK], BF16)
    gyp = [Buf(gyT.ap) for _ in range(KC)]
    zsave = K.sb("zsave", [128, KC, 2], F32)
    zsp = [Buf(zsave.ap) for _ in range(KC)]
    cs = [K.sb("cs", [128, BLK], F32) for _ in range(2)]
    zt = [K.sb("zt", [128, BLK + 2], F32) for _ in range(2)]
    yt = [K.sb("yt", [128, BLK], F32) for _ in range(2)]
    ch = K.sb("ch", [128, 2], F32)
    load_gb(K, g_b, io.conv_g, cj)
    norm_block(K, C, xhalo, 0, 2, g_b, hn, xs, hnTh, hparts, st, [K.ps[6], K.ps[7]])
    it = 0
    for blk in range(NB):
        r0 = blk * BLK
        norm_block(K, C, xin, r0, BLK, g_b, hn, xs, hnT, parts, st, [K.ps[6], K.ps[7]])
        K.qs.dma(out=xacc.ap[:, :, :], in_=xin.ap[r0:r0 + BLK, :].rearrange("(t p) d -> p t d", p=128),
                 r=[xin], w=xflat)
        for dc in range(KC):
            wb = ring.get(io.cwin.ap[cj, dc], [128, KC, 128], io.cwin)
            wc = ring.get(io.cwin.ap[cj, 16 + dc], [128, KC, 128], io.cwin)
            wu = ring.get(io.cwin.ap[cj, 32 + dc], [128, KC, 128], io.cwin)
            psc, psu, psb, psh = K.ps[(it * 3) % 6], K.ps[(it * 3 + 1) % 6], K.ps[(it * 3 + 2) % 6], K.ps[6 + it % 2]
            csb, ztb, ytb = cs[it % 2], zt[it % 2], yt[it % 2]
            it += 1
            for (wt, pb) in ((wc, psc), (wu, psu), (wb, psb)):
                for kc in range(KC):
                    K.T(lambda: nc.tensor.matmul(pb.ap[:, :], wt[1][:, kc, :], hnT.ap[:, kc, :],
                                                 start=(kc == 0), stop=(kc == KC - 1)),
                        r=[wt[0]] + parts, w=[pb])
            if blk == 0:
                for j, wt in enumerate((wc, wu)):
                    for kc in range(KC):
                        K.T(lambda: nc.tensor.matmul(psh.ap[:, j * 2:j * 2 + 2], wt[1][:, kc, :], hnTh.ap[:, kc, 0:2],
                                                     start=(kc == 0), stop=(kc == KC - 1)),
                            r=[wt[0]] + hparts, w=[psh])
            K.A(lambda: nc.scalar.copy(out=csb.ap[:, :], in_=psc.ap[:, :]), r=[psc], w=[csb])
            K.V(lambda: nc.vector.tensor_tensor(out=ztb.ap[:, 2:BLK + 2], in0=psu.ap[:, :], in1=csb.ap[:, :],
                                                op=ALU.mult), r=[psu, csb], w=[ztb])
            if blk == 0:
                K.A(lambda: nc.scalar.copy(out=ch.ap[:, :], in_=psh.ap[:, 0:2]), r=[psh], w=[ch])
                K.V(lambda: nc.vector.tensor_tensor(out=ztb.ap[:, 0:2], in0=psh.ap[:, 2:4], in1=ch.ap[:, :],
                                                    op=ALU.mult), r=[psh, ch], w=[ztb])
                K.V(lambda: nc.vector.tensor_scalar(out=ztb.ap[:, 0:2], in0=ztb.ap[:, 0:2], scalar1=C.mcol.ap[:, 0:1],
                                                    scalar2=None, op0=ALU.mult), r=[C.mcol], w=[ztb])
            else:
                K.V(lambda: nc.vector.tensor_copy(out=ztb.ap[:, 0:2], in_=zsave.ap[:, dc, :]), r=[zsp[dc]], w=[ztb])
            K.V(lambda: nc.vector.tensor_copy(out=zsave.ap[:, dc, :], in_=ztb.ap[:, BLK:BLK + 2]), r=[ztb], w=[zsp[dc]])
            base = cj * 48
            K.V(lambda: nc.vector.tensor_scalar(out=ytb.ap[:, :], in0=ztb.ap[:, 2:BLK + 2],
                                                scalar1=C.cw.ap[:, base + 32 + dc:base + 33 + dc], scalar2=None,
                                                op0=ALU.mult), r=[ztb, C.cw], w=[ytb])
            K.V(lambda: nc.vector.scalar_tensor_tensor(out=ytb.ap[:, :], in0=ztb.ap[:, 1:BLK + 1],
                                                       scalar=C.cw.ap[:, base + 16 + dc:base + 17 + dc],
                                                       in1=ytb.ap[:, :], op0=ALU.mult, op1=ALU.add),
                r=[ztb, C.cw], w=[ytb])
            K.V(lambda: nc.vector.scalar_tensor_tensor(out=ytb.ap[:, :], in0=ztb.ap[:, 0:BLK],
                                                       scalar=C.cw.ap[:, base + dc:base + dc + 1],
                                                       in1=ytb.ap[:, :], op0=ALU.mult, op1=ALU.add),
                r=[ztb, C.cw], w=[ytb])
            K.V(lambda: nc.vector.tensor_tensor(out=gyT.ap[:, dc, :], in0=psb.ap[:, :], in1=ytb.ap[:, :],
                                                op=ALU.mult), r=[psb, ytb], w=[gyp[dc]])
        for dcol in range(8):
            wo = ring.get(io.cwout.ap[cj, dcol], [128, KC, 256], io.cwout)
            for ts in range(4):
                pb = K.ps[(dcol * 4 + ts) % 6]
                for kc in range(KC):
                    K.T(lambda: nc.tensor.matmul(pb.ap[:, 0:256], gyT.ap[:, kc, ts * 128:(ts + 1) * 128], wo[1][:, kc, :],
                                                 start=(kc == 0), stop=(kc == KC - 1)),
                        r=[wo[0]] + gyp, w=[pb])
                K.V(lambda: nc.vector.tensor_tensor(out=xacc.ap[:, ts, dcol * 256:(dcol + 1) * 256],
                                                    in0=pb.ap[:, 0:256],
                                                    in1=xacc.ap[:, ts, dcol * 256:(dcol + 1) * 256], op=ALU.add),
                    r=[pb], w=[xparts[ts][dcol]])
        K.qs2.dma(out=xout.ap[r0:r0 + BLK, :].rearrange("(t p) d -> p t d", p=128), in_=xacc.ap[:, :, :],
                  r=xflat, w=[xout])
    K.end_phase()


def rope_apply(K, src, dst, nh, hd, R, cosb, sinb, tabs, scr, srcbuf, dstbuf):
    nc = K.nc
    s3 = src.rearrange("p (h d) -> p h d", h=nh)
    d3 = dst.rearrange("p (h d) -> p h d", h=nh)
    t = [scr.ap[:, i * 64:i * 64 + nh * R].rearrange("p (h r) -> p h r", h=nh) for i in range(4)]
    x1 = s3[:, :, 0:R]
    x2 = s3[:, :, R:2 * R]
    K.V(lambda: nc.vector.tensor_tensor(out=t[0], in0=x1, in1=cosb, op=ALU.mult), r=[srcbuf, tabs], w=[scr])
    K.V(lambda: nc.vector.tensor_tensor(out=t[1], in0=x2, in1=sinb, op=ALU.mult), r=[srcbuf, tabs], w=[scr])
    K.V(lambda: nc.vector.tensor_tensor(out=t[2], in0=x2, in1=cosb, op=ALU.mult), r=[srcbuf, tabs], w=[scr])
    K.V(lambda: nc.vector.tensor_tensor(out=t[3], in0=x1, in1=sinb, op=ALU.mult), r=[srcbuf, tabs], w=[scr])
    K.V(lambda: nc.vector.tensor_tensor(out=d3[:, :, 0:R], in0=t[0], in1=t[1], op=ALU.subtract), r=[scr], w=[dstbuf])
    K.V(lambda: nc.vector.tensor_tensor(out=d3[:, :, R:2 * R], in0=t[2], in1=t[3], op=ALU.add), r=[scr], w=[dstbuf])
    K.A(lambda: nc.scalar.copy(out=d3[:, :, 2 * R:hd], in_=s3[:, :, 2 * R:hd]), r=[srcbuf], w=[dstbuf])


def attn_half(K, C, io, aj, xown, xoth, xout):
    nc = K.nc
    K.begin_phase()
    ring = Ring(K, 2, 4096)
    KT = K.sb("KT", [128, 4, 4096], BF16)
    Vt = K.sb("Vt", [128, 32, 512], BF16)
    kiT = K.sb("kiT", [128, 4096], BF16)
    HA = K.sb("HA", [128, KC, BLK], BF16)
    QT = K.sb("QT", [128, 16, BLK], BF16)
    qiT = K.sb("qiT", [128, 8, BLK], BF16)
    maskT = K.sb("maskT", [128, 32, 256], BF16)
    R1 = K.sb("R1", [128, 4096], F32)
    R2 = K.sb("R2", [128, 4096], BF16)
    R1c = [Buf(R1.ap) for _ in range(8)]
    g_b_ap = R1.ap[:, 2048:4096]
    xs_ap = R1.ap[:, 0:2048]
    ang_ap = R1.ap[:, 0:768].rearrange("p (s f) -> p s f", s=32)
    ang2_ap = R1.ap[:, 1024:1792].rearrange("p (s f) -> p s f", s=32)
    angi_f = R1.ap[:, 2048:2816].rearrange("p (s f) -> p s f", s=32)
    angi_ap = R1.ap[:, 2048:2816].bitcast(I32).rearrange("p (s f) -> p s f", s=32)
    ang_b, ang2_b, angi_b = R1c[0:2], R1c[2:4], R1c[4:6]
    st = K.sb("st", [128, 8], F32)
    tabs = K.sb("tabs", [128, 2, 32, 24], F32, const=False)
    posi = K.sb("posi", [128, 32], I32)
    inv = K.sb("inv", [128, 24], F32)
    qg_b = K.sb("qg_b", [128, 128], F32)
    kg_b = K.sb("kg_b", [128, 128], F32)
    tri = K.sb("tri", [128, 128], F32)
    ones = K.sb("ones", [128, 128], BF16)
    penm = K.sb("penm", [128, 1], F32)
    pj2 = [K.sb("pj", [128, 512], F32) for _ in range(2)]
    pjb2 = [K.sb("pjb", [128, 512], BF16) for _ in range(2)]
    scr2 = [K.sb("scr", [128, 256], F32) for _ in range(2)]
    sq2 = [K.sb("sq", [128, 512], F32) for _ in range(2)]
    st2 = [K.sb("sth", [128, 4], F32) for _ in range(2)]
    wis = K.sb("wis", [128, 4, 16], F32)
    rbuf = [K.sb("rbuf", [128, 512], BF16) for _ in range(4)]
    ebuf = [K.sb("ebuf", [128, 512], BF16) for _ in range(5)]
    pbuf = [K.sb("pbuf", [128, 512], BF16) for _ in range(5)]
    rden2 = [K.sb("rden", [128, 512], F32) for _ in range(2)]
    bis = K.sb("bis", [128, 40], F32)
    PS = K.ps

    K.qs.dma(out=tri.ap[:, :], in_=io.tri.ap[:, :], r=[io.tri], w=[tri])
    K.V(lambda: nc.vector.memset(ones.ap[:, :], 1.0), w=[ones])
    K.V(lambda: nc.vector.tensor_scalar(out=penm.ap[:, :], in0=C.mcol.ap[:, :], scalar1=-1.0, scalar2=BIG,
                                        op0=ALU.add, op1=ALU.mult), r=[C.mcol], w=[penm])
    K.qs.dma(out=qg_b.ap[:, :], in_=io.q_g.ap[aj:aj + 1, :].partition_broadcast(128), r=[io.q_g], w=[qg_b])
    K.qs.dma(out=kg_b.ap[:, :], in_=io.k_g.ap[aj:aj + 1, :].partition_broadcast(128), r=[io.k_g], w=[kg_b])
    K.qs.dma(out=inv.ap[:, :], in_=io.ropeinv.ap[0:1, :].partition_broadcast(128), r=[io.ropeinv], w=[inv])
    K.qs.dma(out=posi.ap[:, :], in_=io.pos.ap[:, :], r=[io.pos], w=[posi])
    posfT = K.sb("posf", [128, 32], F32)
    K.V(lambda: nc.vector.tensor_copy(out=posfT.ap[:, :], in_=posi.ap[:, :]), r=[posi], w=[posfT])
    K.V(lambda: nc.vector.tensor_tensor(out=ang_ap, in0=posfT.ap[:, :].unsqueeze(2).broadcast_to([128, 32, 24]),
                                        in1=inv.ap[:, :].unsqueeze(1).broadcast_to([128, 32, 24]), op=ALU.mult),
        r=[posfT, inv], w=ang_b)
    TWO_PI = 2.0 * np.pi
    for which in range(2):
        if which == 1:
            K.V(lambda: nc.vector.tensor_scalar(out=ang_ap, in0=ang_ap, scalar1=float(np.pi / 2),
                                                scalar2=None, op0=ALU.add), r=ang_b, w=ang_b)
        K.V(lambda: nc.vector.tensor_scalar(out=ang2_ap, in0=ang_ap, scalar1=float(1.0 / TWO_PI),
                                            scalar2=None, op0=ALU.mult), r=ang_b, w=ang2_b)
        K.V(lambda: nc.vector.tensor_copy(out=angi_ap, in_=ang2_ap), r=ang2_b, w=angi_b)
        K.V(lambda: nc.vector.tensor_copy(out=ang2_ap, in_=angi_ap), r=angi_b, w=ang2_b)
        K.V(lambda: nc.vector.scalar_tensor_tensor(out=ang2_ap, in0=ang2_ap, scalar=float(-TWO_PI),
                                                   in1=ang_ap, op0=ALU.mult, op1=ALU.add),
            r=ang2_b + ang_b, w=ang2_b)
        K.V(lambda: nc.vector.tensor_scalar(out=angi_f, in0=ang2_ap, scalar1=float(np.pi),
                                            scalar2=float(-TWO_PI), op0=ALU.is_gt, op1=ALU.mult), r=ang2_b, w=angi_b)
        K.V(lambda: nc.vector.tensor_tensor(out=ang2_ap, in0=ang2_ap, in1=angi_f,
                                            op=ALU.add), r=ang2_b + angi_b, w=ang2_b)
        K.V(lambda: nc.vector.tensor_scalar(out=angi_f, in0=ang2_ap, scalar1=float(-np.pi),
                                            scalar2=float(TWO_PI), op0=ALU.is_lt, op1=ALU.mult), r=ang2_b, w=angi_b)
        K.V(lambda: nc.vector.tensor_tensor(out=ang2_ap, in0=ang2_ap, in1=angi_f,
                                            op=ALU.add), r=ang2_b + angi_b, w=ang2_b)
        K.V(lambda: nc.vector.tensor_scalar(out=ang2_ap, in0=ang2_ap, scalar1=float(np.pi - 1e-6),
                                            scalar2=float(-np.pi + 1e-6), op0=ALU.min, op1=ALU.max), r=ang2_b, w=ang2_b)
        K.A(lambda: nc.scalar.activation(out=tabs.ap[:, which, :, :], in_=ang2_ap, func=AF.Sin),
            r=ang2_b, w=[tabs])

    def tab(which, sub, off, R, nh):
        return tabs.ap[:, which, sub, off:off + R].unsqueeze(1).broadcast_to([128, nh, R])

    ntp = [0]

    def transpose_to(src_ap, srcbuf, dst_ap, dstbuf, nrows=128):
        pb = PS[2 + ntp[0] % 2]
        ntp[0] += 1
        pv = bf16v(pb)
        K.T(lambda: nc.tensor.transpose(pv[0:nrows, 0:128], src_ap, C.ident.ap[:, :]), r=[srcbuf, C.ident], w=[pb])
        K.A(lambda: nc.scalar.copy(out=dst_ap, in_=pv[0:nrows, 0:128]), r=[pb], w=[dstbuf])

    nproj = [0]
    nix = [0]
    nout = [0]
    QTs = [Buf(QT.ap) for _ in range(4)]
    gsrc = io.attn_g

    for blk8 in range(8):
        own = blk8 >= 4
        blk = blk8 % 4
        r0 = blk * BLK
        K.qs.dma(out=g_b_ap, in_=gsrc.ap[aj:aj + 1, :].partition_broadcast(128), r=[gsrc], w=R1c[4:8])
        nsub = 4
        for ts in range(nsub):
            if own:
                xsb, xsa = xown, xown.ap[r0 + ts * 128: r0 + (ts + 1) * 128, :]
            else:
                xsb, xsa = xoth(r0 + ts * 128)
            K.qs.dma(out=xs_ap, in_=xsa, r=[xsb], w=R1c[0:4])
            K.A(lambda: nc.scalar.activation(out=R2.ap[:, 0:2048], in_=xs_ap, func=AF.Square,
                                             accum_out=st.ap[:, 0:1]), r=R1c[0:4], w=[R2, st])
            K.A(lambda: nc.scalar.activation(out=st.ap[:, 1:2], in_=st.ap[:, 0:1], func=AF.Sqrt,
                                             scale=1.0 / D, bias=C.eps.ap[:, 0:1]), r=[st, C.eps], w=[st])
            K.V(lambda: nc.vector.reciprocal(out=st.ap[:, 2:3], in_=st.ap[:, 1:2]), r=[st], w=[st])
            K.V(lambda: nc.vector.scalar_tensor_tensor(out=R2.ap[:, 0:2048], in0=xs_ap, scalar=st.ap[:, 2:3],
                                                       in1=g_b_ap, op0=ALU.mult, op1=ALU.mult),
                r=R1c + [st], w=[R2])
            for half in range(2):
                pb = PS[2 + half]
                pv = bf16v(pb)
                for k in range(8):
                    kc = half * 8 + k
                    K.T(lambda: nc.tensor.transpose(pv[:, k * 128:(k + 1) * 128], R2.ap[:, kc * 128:(kc + 1) * 128],
                                                    C.ident.ap[:, :]), r=[R2, C.ident], w=[pb])
                dst = HA.ap[:, half * 8:(half + 1) * 8, ts * 128:(ts + 1) * 128]
                src = pv.rearrange("p (k t) -> p k t", k=8)
                if half == 0:
                    K.A(lambda: nc.scalar.copy(out=dst, in_=src), r=[pb], w=[HA])
                else:
                    K.V(lambda: nc.vector.tensor_copy(out=dst, in_=src), r=[pb], w=[HA])
        sub0 = (16 if own else 0) + blk * 4
        tok0 = (2048 if own else 0) + r0

        def run_proj(wap, ncol, tp):
            ws, wv = wap
            pb = PS[nproj[0] % 2]
            nproj[0] += 1
            for t2 in range(2):
                ts = 2 * tp + t2
                for kc in range(KC):
                    K.T(lambda: nc.tensor.matmul(pb.ap[:, t2 * ncol:(t2 + 1) * ncol], HA.ap[:, kc, ts * 128:(ts + 1) * 128],
                                                 wv[:, kc, :], start=(kc == 0), stop=(kc == KC - 1)), r=[ws, HA], w=[pb])
            par = nproj[0] % 2
            return pb, pj2[par], pjb2[par], scr2[par], sq2[par], st2[par]

        def headnorm(pb, gbuf, pj, sq, sth):
            pb3 = pb.ap[:, 0:512].rearrange("p (g d) -> p g d", g=4)
            pj3 = pj.ap[:, 0:512].rearrange("p (g d) -> p g d", g=4)
            K.A(lambda: nc.scalar.activation(out=sq.ap[:, 0:512], in_=pb.ap[:, 0:512], func=AF.Square), r=[pb], w=[sq])
            K.V(lambda: nc.vector.tensor_reduce(out=sth.ap[:, 0:4], in_=sq.ap[:, 0:512].rearrange("p (g d) -> p g d", g=4),
                                                axis=AX.X, op=ALU.add), r=[sq], w=[sth])
            K.A(lambda: nc.scalar.activation(out=sth.ap[:, 0:4], in_=sth.ap[:, 0:4], func=AF.Sqrt,
                                             scale=1.0 / 128, bias=C.eps.ap[:, 0:1]), r=[sth, C.eps], w=[sth])
            K.V(lambda: nc.vector.reciprocal(out=sth.ap[:, 0:4], in_=sth.ap[:, 0:4]), r=[sth], w=[sth])
            K.V(lambda: nc.vector.tensor_tensor(out=pj3, in0=pb3, in1=sth.ap[:, 0:4].unsqueeze(2).broadcast_to([128, 4, 128]),
                                                op=ALU.mult), r=[pb, sth], w=[pj])
            K.V(lambda: nc.vector.tensor_tensor(out=pj3, in0=pj3, in1=gbuf.ap[:, :].unsqueeze(1).broadcast_to([128, 4, 128]),
                                                op=ALU.mult), r=[gbuf], w=[pj])

        def rope2(pj, pjb, scr, nh, hd, R, off, sub):
            n = 2 * nh * hd
            s4 = pj.ap[:, 0:n].rearrange("p (t h d) -> p t h d", t=2, h=nh)
            d4 = pjb.ap[:, 0:n].rearrange("p (t h d) -> p t h d", t=2, h=nh)
            cosb = tabs.ap[:, 1, sub:sub + 2, off:off + R].unsqueeze(2).broadcast_to([128, 2, nh, R])
            sinb = tabs.ap[:, 0, sub:sub + 2, off:off + R].unsqueeze(2).broadcast_to([128, 2, nh, R])
            t = [scr.ap[:, i * 64:i * 64 + 2 * nh * R].rearrange("p (t h r) -> p t h r", t=2, h=nh) for i in range(4)]
            x1 = s4[:, :, :, 0:R]
            x2 = s4[:, :, :, R:2 * R]
            K.V(lambda: nc.vector.tensor_tensor(out=t[0], in0=x1, in1=cosb, op=ALU.mult), r=[pj, tabs], w=[scr])
            K.V(lambda: nc.vector.tensor_tensor(out=t[1], in0=x2, in1=sinb, op=ALU.mult), r=[pj, tabs], w=[scr])
            K.V(lambda: nc.vector.tensor_tensor(out=t[2], in0=x2, in1=cosb, op=ALU.mult), r=[pj, tabs], w=[scr])
            K.V(lambda: nc.vector.tensor_tensor(out=t[3], in0=x1, in1=sinb, op=ALU.mult), r=[pj, tabs], w=[scr])
            K.V(lambda: nc.vector.tensor_tensor(out=d4[:, :, :, 0:R], in0=t[0], in1=t[1], op=ALU.subtract), r=[scr], w=[pjb])
            K.V(lambda: nc.vector.tensor_tensor(out=d4[:, :, :, R:2 * R], in0=t[2], in1=t[3], op=ALU.add), r=[scr], w=[pjb])
            K.A(lambda: nc.scalar.copy(out=d4[:, :, :, 2 * R:hd], in_=s4[:, :, :, 2 * R:hd]), r=[pj], w=[pjb])

        def transpose2(pjb, ncol, c0, dst_ap, dstbuf):
            pb = PS[2 + ntp[0] % 2]
            ntp[0] += 1
            pv = bf16v(pb)
            for t2 in range(2):
                K.T(lambda: nc.tensor.transpose(pv[:, t2 * 128:(t2 + 1) * 128],
                                                pjb.ap[:, t2 * ncol + c0:t2 * ncol + c0 + 128], C.ident.ap[:, :]),
                    r=[pjb, C.ident], w=[pb])
            K.A(lambda: nc.scalar.copy(out=dst_ap, in_=pv[:, 0:256]), r=[pb], w=[dstbuf])

        for cb in range(2):
            wk = ring.get(io.wkv.ap[aj, cb], [128, KC, 256], io.wkv)
            for tp in range(2):
                pb, pj, pjb, scr, sq, sth = run_proj(wk, 256, tp)
                headnorm(pb, kg_b, pj, sq, sth)
                rope2(pj, pjb, scr, 2, 128, 16, 0, sub0 + 2 * tp)
                for h in range(2):
                    transpose2(pjb, 256, h * 128, KT.ap[:, cb * 2 + h, tok0 + tp * 256: tok0 + (tp + 1) * 256], KT)
        for cb in range(2):
            wv_ = ring.get(io.wkv.ap[aj, 2 + cb], [128, KC, 256], io.wkv)
            for tp in range(2):
                pb = run_proj(wv_, 256, tp)[0]
                kt = tok0 // 128 + 2 * tp
                K.A(lambda: nc.scalar.copy(out=Vt.ap[:, kt:kt + 2, cb * 256:(cb + 1) * 256],
                                           in_=pb.ap[:, 0:512].rearrange("p (t c) -> p t c", t=2)), r=[pb], w=[Vt])
        wki = ring.get(io.wki.ap[aj], [128, KC, 128], io.wki)
        for tp in range(2):
            pb, pj, pjb, scr, sq, sth = run_proj(wki, 128, tp)
            K.A(lambda: nc.scalar.copy(out=pj.ap[:, 0:256], in_=pb.ap[:, 0:256]), r=[pb], w=[pj])
            rope2(pj, pjb, scr, 2, 64, 8, 16, sub0 + 2 * tp)
            transpose2(pjb, 128, 0, kiT.ap[:, tok0 + tp * 256: tok0 + (tp + 1) * 256], kiT)
        if not own:
            continue
        for cb in range(8):
            wq = ring.get(io.wq.ap[aj, cb], [128, KC, 256], io.wq)
            for tp in range(2):
                pb, pj, pjb, scr, sq, sth = run_proj(wq, 256, tp)
                headnorm(pb, qg_b, pj, sq, sth)
                rope2(pj, pjb, scr, 2, 128, 16, 0, sub0 + 2 * tp)
                for h in range(2):
                    transpose2(pjb, 256, h * 128, QT.ap[:, cb * 2 + h, tp * 256:(tp + 1) * 256], QT)
        for cb in range(4):
            wqi = ring.get(io.wqi.ap[aj, cb], [128, KC, 256], io.wqi)
            for tp in range(2):
                pb, pj, pjb, scr, sq, sth = run_proj(wqi, 256, tp)
                K.A(lambda: nc.scalar.copy(out=pj.ap[:, 0:512], in_=pb.ap[:, 0:512]), r=[pb], w=[pj])
                rope2(pj, pjb, scr, 4, 64, 8, 16, sub0 + 2 * tp)
                for h2 in range(2):
                    transpose2(pjb, 256, h2 * 128, qiT.ap[:, cb * 2 + h2, tp * 256:(tp + 1) * 256], qiT)
        wwi = ring.get(io.wwi.ap[aj], [128, KC, 16], io.wwi)
        for tp in range(2):
            pb = run_proj(wwi, 16, tp)[0]
            K.V(lambda: nc.vector.tensor_scalar(out=wis.ap[:, 2 * tp:2 * tp + 2, :],
                                                in0=pb.ap[:, 0:32].rearrange("p (t c) -> p t c", t=2), scalar1=1.0 / 32.0,
                                                scalar2=None, op0=ALU.mult), r=[pb], w=[wis])

        for qh in range(2):
            nkt = 16 + blk * 4 + qh * 2 + 2
            for jj in range(2):
                j = qh * 2 + jj
                jt = blk * 4 + j
                NK = 2048 + (jt + 1) * 128
                sc = R1
                nch = (NK + 511) // 512
                scr_ = R1c[0:nch]
                IB = [PS[0], PS[1], PS[4], PS[5]]
                for h in range(16):
                    lo = (h % 2) * 64
                    for c in range(nch):
                        cw_ = min(512, NK - c * 512)
                        pb = IB[nix[0] % 4]
                        rb = rbuf[nix[0] % 4]
                        nix[0] += 1
                        K.T(lambda: nc.tensor.matmul(pb.ap[:, 0:cw_], qiT.ap[lo:lo + 64, h // 2, j * 128:(j + 1) * 128],
                                                     kiT.ap[lo:lo + 64, c * 512:c * 512 + cw_], start=True, stop=True),
                            r=[qiT, kiT], w=[pb])
                        K.A(lambda: nc.scalar.activation(out=rb.ap[:, 0:cw_], in_=pb.ap[:, 0:cw_], func=AF.Relu),
                            r=[pb], w=[rb])
                        if h == 0:
                            K.V(lambda: nc.vector.tensor_scalar(out=sc.ap[:, c * 512:c * 512 + cw_], in0=rb.ap[:, 0:cw_],
                                                                scalar1=wis.ap[:, j, 0:1], scalar2=None, op0=ALU.mult),
                                r=[rb, wis], w=[R1c[c]])
                        else:
                            K.V(lambda: nc.vector.scalar_tensor_tensor(out=sc.ap[:, c * 512:c * 512 + cw_],
                                                                       in0=rb.ap[:, 0:cw_], scalar=wis.ap[:, j, h:h + 1],
                                                                       in1=sc.ap[:, c * 512:c * 512 + cw_],
                                                                       op0=ALU.mult, op1=ALU.add),
                                r=[rb, wis], w=[R1c[c]])
                K.V(lambda: nc.vector.tensor_reduce(out=bis.ap[:, 0:1], in_=sc.ap[:, 0:NK], axis=AX.X, op=ALU.min),
                    r=scr_, w=[bis])
                K.V(lambda: nc.vector.tensor_scalar(out=sc.ap[:, 0:2048], in0=sc.ap[:, 0:2048], scalar1=penm.ap[:, 0:1],
                                                    scalar2=None, op0=ALU.add), r=[penm], w=R1c[0:4])
                K.V(lambda: nc.vector.tensor_tensor(out=sc.ap[:, NK - 128:NK], in0=sc.ap[:, NK - 128:NK], in1=tri.ap[:, :],
                                                    op=ALU.add), r=[tri], w=[R1c[(NK - 128) // 512]])
                K.V(lambda: nc.vector.tensor_reduce(out=bis.ap[:, 1:2], in_=sc.ap[:, 0:NK], axis=AX.X, op=ALU.max),
                    r=scr_, w=[bis])
                K.V(lambda: nc.vector.tensor_scalar(out=bis.ap[:, 0:1], in0=bis.ap[:, 0:1], scalar1=-1.0, scalar2=None,
                                                    op0=ALU.add), r=[bis], w=[bis])
                K.V(lambda: nc.vector.scalar_tensor_tensor(out=bis.ap[:, 2:3], in0=bis.ap[:, 0:1], scalar=-1.0,
                                                           in1=bis.ap[:, 1:2], op0=ALU.mult, op1=ALU.add),
                    r=[bis], w=[bis])
                K.V(lambda: nc.vector.tensor_scalar(out=bis.ap[:, 2:3], in0=bis.ap[:, 2:3], scalar1=1e-3, scalar2=None,
                                                    op0=ALU.add), r=[bis], w=[bis])
                for k in range(NBIS + 1):
                    K.V(lambda: nc.vector.tensor_scalar(out=bis.ap[:, 8 + k:9 + k], in0=bis.ap[:, 2:3],
                                                        scalar1=float(2.0 ** -(k + 1)), scalar2=None, op0=ALU.mult),
                        r=[bis], w=[bis])
                K.V(lambda: nc.vector.tensor_tensor(out=bis.ap[:, 3:4], in0=bis.ap[:, 0:1], in1=bis.ap[:, 8:9], op=ALU.add),
                    r=[bis], w=[bis])
                for k in range(NBIS):
                    K.V(lambda: nc.vector.tensor_scalar(out=R2.ap[:, 0:NK], in0=sc.ap[:, 0:NK], scalar1=bis.ap[:, 3:4],
                                                        scalar2=None, op0=ALU.is_ge, op1=ALU.add,
                                                        accum_out=bis.ap[:, 4:5]), r=scr_ + [bis], w=[R2, bis])
                    K.V(lambda: nc.vector.tensor_scalar(out=bis.ap[:, 5:6], in0=bis.ap[:, 4:5], scalar1=TOPK - 0.5,
                                                        scalar2=0.5, op0=ALU.is_ge, op1=ALU.subtract), r=[bis], w=[bis])
                    K.V(lambda: nc.vector.scalar_tensor_tensor(out=bis.ap[:, 3:4], in0=bis.ap[:, 5:6],
                                                               scalar=bis.ap[:, 8 + k:9 + k], in1=bis.ap[:, 3:4],
                                                               op0=ALU.mult, op1=ALU.add), r=[bis], w=[bis])
                K.V(lambda: nc.vector.tensor_tensor(out=bis.ap[:, 6:7], in0=bis.ap[:, 3:4], in1=bis.ap[:, 8 + NBIS:9 + NBIS],
                                                    op=ALU.subtract), r=[bis], w=[bis])
                K.V(lambda: nc.vector.tensor_scalar(out=R2.ap[:, 0:NK], in0=sc.ap[:, 0:NK], scalar1=bis.ap[:, 6:7],
                                                    scalar2=None, op0=ALU.is_ge), r=scr_ + [bis], w=[R2])
                nk_t = NK // 128
                for k0 in range(0, nk_t, 8):
                    kn = min(8, nk_t - k0)
                    pb = PS[2 + (k0 // 8) % 2]
                    pv = bf16v(pb)
                    for k in range(kn):
                        K.T(lambda: nc.tensor.transpose(pv[:, k * 128:(k + 1) * 128],
                                                        R2.ap[:, (k0 + k) * 128:(k0 + k + 1) * 128], C.ident.ap[:, :]),
                            r=[R2, C.ident], w=[pb])
                    K.A(lambda: nc.scalar.copy(out=maskT.ap[:, k0:k0 + kn, jj * 128:(jj + 1) * 128],
                                               in_=pv[:, 0:kn * 128].rearrange("p (k t) -> p k t", k=kn)),
                        r=[pb], w=[maskT])
                if nk_t < nkt:
                    K.V(lambda: nc.vector.memset(maskT.ap[:, nk_t:nkt, jj * 128:(jj + 1) * 128], 0.0), w=[maskT])
            q0 = qh * 256
            units = [(hp, kt) for hp in range(8) for kt in range(nkt)]
            SB_ = [PS[4], PS[5], PS[1], PS[0]]
            LOOK = 3

            def emit_s(u):
                hp, kt = units[u]
                kvh = hp // 2
                psS = SB_[u % 4]
                eb = ebuf[u % 5]
                pbf = pbuf[u % 5]
                K.T(lambda: nc.tensor.matmul(psS.ap[:, 0:512], KT.ap[:, kvh, kt * 128:(kt + 1) * 128],
                                             QT.ap[:, 2 * hp:2 * hp + 2, q0:q0 + 256], start=True, stop=True),
                    r=[KT, QT], w=[psS])
                K.A(lambda: nc.scalar.activation(out=eb.ap[:, :], in_=psS.ap[:, :], func=AF.Exp,
                                                 scale=float(128 ** -0.5)), r=[psS], w=[eb])
                K.V(lambda: nc.vector.tensor_tensor(out=pbf.ap[:, :].rearrange("p (k t) -> p k t", k=2),
                                                    in0=eb.ap[:, :].rearrange("p (k t) -> p k t", k=2),
                                                    in1=maskT.ap[:, kt, :].unsqueeze(1).broadcast_to([128, 2, 256]),
                                                    op=ALU.mult), r=[eb, maskT], w=[pbf])

            def emit_od(u):
                hp, kt = units[u]
                kvh = hp // 2
                pbf = pbuf[u % 5]
                psO, psD = (PS[6], PS[7]) if hp % 2 == 0 else (PS[2], PS[3])
                first = (kt == 0)
                last = (kt == nkt - 1)
                K.T(lambda: nc.tensor.matmul(psO.ap[:, 0:512], Vt.ap[:, kt, kvh * 128:(kvh + 1) * 128],
                                             pbf.ap[:, 0:512], start=first, stop=last), r=[Vt, pbf], w=[psO])
                K.T(lambda: nc.tensor.matmul(psD.ap[:, 0:512], ones.ap[:, :], pbf.ap[:, 0:512],
                                             start=first, stop=last), r=[ones, pbf], w=[psD])
                if last:
                    rden = rden2[hp % 2]
                    K.V(lambda: nc.vector.reciprocal(out=rden.ap[:, :], in_=psD.ap[:, 0:512]), r=[psD], w=[rden])
                    K.V(lambda: nc.vector.tensor_tensor(out=HA.ap[:, 2 * hp:2 * hp + 2, q0:q0 + 256],
                                                        in0=psO.ap[:, 0:512].rearrange("p (k t) -> p k t", k=2),
                                                        in1=rden.ap[:, :].rearrange("p (k t) -> p k t", k=2),
                                                        op=ALU.mult), r=[psO, rden], w=[HA])

            for u in range(len(units) + LOOK):
                if u < len(units):
                    emit_s(u)
                if u >= LOOK:
                    emit_od(u - LOOK)
        xslots = [(QTs[0], QT, QT.ap[:, 0:8, :].rearrange("p a (b c) -> p (a b) c", c=256)),
                  (QTs[1], QT, QT.ap[:, 8:16, :].rearrange("p a (b c) -> p (a b) c", c=256)),
                  (QTs[2], qiT, qiT.ap[:, :, :].rearrange("p a (b c) -> p (a b) c", c=256))]
        for sbq, par_, _v in xslots:
            sbq.w = dict(par_.w)
            sbq.r = dict(par_.r)
        xs2 = R1.ap[:, :].rearrange("p (t d) -> p t d", t=2)
        for tpair in range(2):
            rr = r0 + tpair * 256
            K.qs.dma(out=xs2, in_=xown.ap[rr:rr + 256, :].rearrange("(t p) d -> p t d", p=128), r=[xown], w=R1c)
            for dcol in range(8):
                k6 = nout[0] % 5
                nout[0] += 1
                if k6 < 2:
                    wo = ring.get(io.wao.ap[aj, dcol], [128, KC, 256], io.wao)
                else:
                    sbq, par_, vq = xslots[k6 - 2]
                    K.qg.dma(out=vq, in_=io.wao.ap[aj, dcol], r=[io.wao], w=[sbq])
                    wo = (sbq, vq)
                for t in range(2):
                    ts = 2 * tpair + t
                    pb = PS[(dcol * 2 + t) % 2]
                    for h in range(16):
                        K.T(lambda: nc.tensor.matmul(pb.ap[:, 0:256], HA.ap[:, h, ts * 128:(ts + 1) * 128], wo[1][:, h, :],
                                                     start=(h == 0), stop=(h == 15)), r=[wo[0], HA], w=[pb])
                    c0 = t * 2048 + dcol * 256
                    K.V(lambda: nc.vector.tensor_tensor(out=R1.ap[:, c0:c0 + 256], in0=pb.ap[:, 0:256],
                                                        in1=R1.ap[:, c0:c0 + 256], op=ALU.add),
                        r=[pb], w=[R1c[c0 // 512]])
            K.qs2.dma(out=xout.ap[rr:rr + 256, :].rearrange("(t p) d -> p t d", p=128), in_=xs2, r=R1c, w=[xout])
        for sbq, par_, _v in xslots:
            for tk in list(sbq.w.values()) + list(sbq.r.values()):
                _merge(par_.r, tk)
    K.end_phase()


PAIRS = [[0, 1], [2, 3], [4, 5], [6, 7]]


def allgather(K, src, dst):
    E = K.pool
    sem = K.newsem(K.uid("cc"))
    E.wait(E.deps([src], [dst]))
    ins = K.nc.gpsimd.collective_compute("AllGather", ALU.bypass, replica_groups=PAIRS,
                                         ins=[src.ap.ap().opt()], outs=[dst.ap.ap().opt()])
    ins.then_inc(sem)
    t = ('d', sem, 1)
    _merge(src.r, t)
    dst.w = {}
    dst.r = {}
    _merge(dst.w, t)


def declare_io(K):
    io = Ctx()
    ext = "ExternalInput"
    io.ident = K.dram("ident", [128, 128], F32, ext)
    io.halo_m = K.dram("halo_m", [128, 1], F32, ext)
    io.convw = K.dram("convw", [128, 96], F32, ext)
    io.xown = K.dram("xown", [TOK, D], F32, ext)
    io.xoth = K.dram("xoth", [TOK, D], F32, ext)
    io.mlp_g = K.dram("mlp_g", [4, D], F32, ext)
    io.w1 = K.dram("w1", [4, NFC, 128, KC, 128], F32, ext)
    io.w2 = K.dram("w2", [4, NFC, 128, D], F32, ext)
    io.tri = K.dram("tri", [128, 128], F32, ext)
    io.ropeinv = K.dram("ropeinv", [1, 24], F32, ext)
    io.pos = K.dram("pos", [128, 32], I32, ext)
    io.attn_g = K.dram("attn_g", [2, D], F32, ext)
    io.q_g = K.dram("q_g", [2, 128], F32, ext)
    io.k_g = K.dram("k_g", [2, 128], F32, ext)
    io.wq = K.dram("wq", [2, 8, 128, KC, 256], F32, ext)
    io.wkv = K.dram("wkv", [2, 4, 128, KC, 256], F32, ext)
    io.wki = K.dram("wki", [2, 128, KC, 128], F32, ext)
    io.wqi = K.dram("wqi", [2, 4, 128, KC, 256], F32, ext)
    io.wwi = K.dram("wwi", [2, 128, KC, 16], F32, ext)
    io.wao = K.dram("wao", [2, 8, 128, KC, 256], F32, ext)
    io.conv_g = K.dram("conv_g", [2, D], F32, ext)
    io.cwin = K.dram("cwin", [2, 48, 128, KC, 128], F32, ext)
    io.cwout = K.dram("cwout", [2, 8, 128, KC, 256], F32, ext)
    io.y = K.dram("y", [TOK, D], F32, "ExternalOutput")
    return io


def build_fused():
    K = Kern()
    C = Ctx()
    io = declare_io(K)
    setup_consts(K, C, io)
    xcur = io.xown
    xoth = lambda r: (io.xoth, io.xoth.ap[r:r + 128, :])
    for li in range(4):
        j = li // 2
        xmid = K.dram("xmid%d" % li, [TOK, D], F32)
        if li % 2 == 0:
            attn_half(K, C, io, j, xcur, xoth, xmid)
        else:
            hb = K.dram("hb%d" % li, [2, D], F32)
            hall = K.dram("hall%d" % li, [4, D], F32)
            K.qs.dma(out=hb.ap[:, :], in_=xcur.ap[TOK - 2:TOK, :], r=[xcur], w=[hb])
            allgather(K, hb, hall)
            conv_half(K, C, io, j, xcur, hall, xmid)
        xnext = io.y if li == 3 else K.dram("xr%d" % li, [TOK, D], F32)
        hook = None
        if li == 1:
            ags = [K.dram("ags%d" % i, [128, D], F32) for i in range(16)]
            agd = [K.dram("agd%d" % i, [256, D], F32) for i in range(16)]

            def hook(blk, xnext=xnext, ags=ags, agd=agd):
                for t in range(4):
                    i = blk * 4 + t
                    K.qs.dma(out=ags[i].ap[:, :], in_=xnext.ap[i * 128:(i + 1) * 128, :], r=[xnext], w=[ags[i]])
                    allgather(K, ags[i], agd[i])
            xoth = lambda r, agd=agd: (agd[r // 128], agd[r // 128].ap[0:128, :])
        mlp_half(K, C, io, li, xmid, xnext, after_store=hook)
        xcur = xnext
    K.barrier()
    return K.nc


def colblocks(W, bw):
    Cc = W.shape[1]
    return np.ascontiguousarray(W.reshape(KC, 128, Cc // bw, bw).transpose(2, 1, 0, 3))


_PROG = []


def kernel(x, positions, attn_norm_g, attn_w_in, attn_q_norm_g, attn_k_norm_g, attn_w_out,
           conv_norm_g, conv_w_in, conv_w, conv_w_out, mlp_norm_g, mlp_w1, mlp_w2):
    f32 = np.float32
    x = np.asarray(x, f32)
    positions = np.asarray(positions, np.int32)
    ident = np.eye(128, dtype=f32)
    tri = np.where(np.arange(128)[None, :] <= np.arange(128)[:, None], 0.0, -BIG).astype(f32)
    inv_main = (ROPE_THETA ** (-(np.arange(0, 32, 2, dtype=f32)) / f32(32))).astype(f32)
    inv_idx = (ROPE_THETA ** (-(np.arange(0, 16, 2, dtype=f32)) / f32(16))).astype(f32)
    ropeinv = np.concatenate([inv_main, inv_idx])[None, :].astype(f32)
    conv_w = np.asarray(conv_w, f32)
    convw = np.ascontiguousarray(conv_w.reshape(2, 3, 16, 128).transpose(3, 0, 1, 2).reshape(128, 96))
    xc = x.reshape(8, TOK, D)
    w1 = np.stack([colblocks(np.asarray(mlp_w1[l], f32), 128) for l in range(4)])
    w2 = np.ascontiguousarray(np.asarray(mlp_w2, f32).reshape(4, NFC, 128, D))
    wq, wkv, wqi, wki, wwi, wao = [], [], [], [], [], []
    for j in range(2):
        W = np.asarray(attn_w_in[j], f32)
        wq.append(colblocks(W[:, 0:2048], 256))
        wkv.append(colblocks(W[:, 2048:3072], 256))
        wqi.append(colblocks(W[:, 3072:4096], 256))
        kicols = W[:, 4096:4160]
        wki.append(colblocks(np.concatenate([kicols, kicols], axis=1), 128)[0])
        wwi.append(colblocks(W[:, 4160:4176], 16)[0])
        wao.append(colblocks(np.asarray(attn_w_out[j], f32), 256))
    cwin = np.stack([colblocks(np.asarray(conv_w_in[j], f32), 128) for j in range(2)])
    cwout = np.stack([colblocks(np.asarray(conv_w_out[j], f32), 256) for j in range(2)])
    shared = {
        "ident": ident, "convw": convw, "tri": tri, "ropeinv": ropeinv,
        "mlp_g": np.ascontiguousarray(np.asarray(mlp_norm_g, f32)),
        "attn_g": np.ascontiguousarray(np.asarray(attn_norm_g, f32)),
        "conv_g": np.ascontiguousarray(np.asarray(conv_norm_g, f32)),
        "q_g": np.ascontiguousarray(np.asarray(attn_q_norm_g, f32)),
        "k_g": np.ascontiguousarray(np.asarray(attn_k_norm_g, f32)),
        "w1": w1, "w2": w2, "wq": np.stack(wq), "wkv": np.stack(wkv), "wqi": np.stack(wqi),
        "wki": np.stack(wki), "wwi": np.stack(wwi), "wao": np.stack(wao), "cwin": cwin, "cwout": cwout,
    }
    in_maps = []
    for c in range(8):
        b, hf = c // 2, c % 2
        p_oth = positions[b, 0:TOK].reshape(16, 128).T
        p_own = positions[b, hf * TOK:(hf + 1) * TOK].reshape(16, 128).T
        m = dict(shared)
        m["xown"] = np.ascontiguousarray(xc[c])
        m["xoth"] = np.ascontiguousarray(xc[2 * b])
        m["pos"] = np.ascontiguousarray(np.concatenate([p_oth, p_own], axis=1).astype(np.int32))
        m["halo_m"] = np.full((128, 1), float(hf), f32)
        in_maps.append(m)
    if not _PROG:
        _PROG.append(build_fused())
    res = run_bass_kernel_spmd(_PROG[0], in_maps, core_ids=list(range(8)))
    out = np.stack([np.asarray(res.results[c]["y"], f32) for c in range(8)], 0).reshape(4, 4096, D)
    return out
```

```python
import bisect
import contextlib
import numpy as np
import concourse.bass as bass
import concourse.mybir as mybir
from concourse.bass_utils import run_bass_kernel_spmd

F32 = mybir.dt.float32
BF16 = mybir.dt.bfloat16
I32 = mybir.dt.int32
AF = mybir.ActivationFunctionType
ALU = mybir.AluOpType
AX = mybir.AxisListType

D = 2048
KC = 16
TOK = 2048
BLK = 512
NB = TOK // BLK
DFF = 8192
NFC = DFF // 128
EPS = 1e-6
BIG = 30000.0
TOPK = 256
NBIS = 12
G = 4
ROPE_THETA = 500000.0


class Buf:
    def __init__(self, ap, const=False):
        self.ap = ap
        self.w = {}
        self.r = {}
        self.const = const

    def __getitem__(self, k):
        return self.ap[k]


def _merge(d, t):
    if t[0] == 'e':
        key = ('e', id(t[1]))
        if key not in d or d[key][2] < t[2]:
            d[key] = t
    else:
        key = ('d', t[1].num)
        if key not in d or d[key][2] < t[2]:
            d[key] = t


class Eng:
    def __init__(self, K, e, name, is_pe=False):
        self.K = K
        self.e = e
        self.sem = K.newsem("s_" + name)
        self.val = 0
        self.seq = 0
        self.last = None
        self.sigseq = []
        self.sigval = []
        self.seen = {}
        self.is_pe = is_pe
        self.eager = False

    def value_for(self, seq):
        i = bisect.bisect_left(self.sigseq, seq)
        if i < len(self.sigseq):
            return self.sigval[i]
        assert self.last is not None and self.seq >= seq
        self.last.then_inc(self.sem, 1)
        self.val += 1
        self.sigseq.append(self.seq)
        self.sigval.append(self.val)
        return self.val

    def wait(self, tks):
        for t in tks:
            if t[0] == 'e':
                src = t[1]
                if src is self and self.is_pe:
                    continue
                sem = src.sem
                v = src.value_for(t[2])
            else:
                sem, v = t[1], t[2]
            if self.seen.get(sem.num, 0) >= v:
                continue
            self.e.wait_ge(sem, v)
            self.seen[sem.num] = v

    def deps(self, reads, writes):
        d = []
        for b in reads:
            d.extend(b.w.values())
        for b in writes:
            d.extend(b.w.values())
            d.extend(b.r.values())
        return d

    def op(self, fn, r=(), w=()):
        self.wait(self.deps(r, w))
        ins = fn()
        self.seq += 1
        self.last = ins
        if self.eager:
            ins.then_inc(self.sem, 1)
            self.val += 1
            self.sigseq.append(self.seq)
            self.sigval.append(self.val)
        t = ('e', self, self.seq)
        for b in r:
            if not b.const:
                _merge(b.r, t)
        for b in w:
            b.w = {}
            b.r = {}
            _merge(b.w, t)
        return t


class DmaQ:
    def __init__(self, K, eng, name, nsem):
        self.eng = eng
        self.sems = [K.newsem("d_%s%d" % (name, i)) for i in range(nsem)]
        self.vals = [0] * nsem
        self.i = 0

    def dma(self, out, in_, r=(), w=()):
        E = self.eng
        k = self.i % len(self.sems)
        self.i += 1
        d = E.deps(r, w)
        if self.vals[k] > 0:
            d.append(('d', self.sems[k], self.vals[k]))
        E.wait(d)
        ins = E.e.dma_start(out=out, in_=in_)
        ins.then_inc(self.sems[k], 16)
        self.vals[k] += 16
        t = ('d', self.sems[k], self.vals[k])
        for b in r:
            if not b.const:
                _merge(b.r, t)
        for b in w:
            b.w = {}
            b.r = {}
            _merge(b.w, t)
        return t


class Kern:
    def __init__(self):
        self.nc = bass.Bass("TRN2", target_bir_lowering=False)
        self.es = contextlib.ExitStack()
        self.phase = None
        nc = self.nc
        self.pe = Eng(self, nc.tensor, "pe", is_pe=True)
        self.act = Eng(self, nc.scalar, "act")
        self.dve = Eng(self, nc.vector, "dve")
        self.pool = Eng(self, nc.gpsimd, "pool")
        self.sp = Eng(self, nc.sync, "sp")
        self.engs = [self.pe, self.act, self.dve, self.pool, self.sp]
        self.act.eager = True
        self.dve.eager = True
        self.qs = DmaQ(self, self.sp, "sp", 10)
        self.qg = DmaQ(self, self.pool, "pl", 10)
        self.qs2 = DmaQ(self, self.sp, "so", 4)
        self.n_uid = 0
        self.ps = []
        for i in range(8):
            t = self.es.enter_context(nc.psum_tensor("ps%d" % i, [128, 512], F32))
            self.ps.append(Buf(t))

    def newsem(self, name):
        return self.es.enter_context(self.nc.semaphore(name))

    def uid(self, p):
        self.n_uid += 1
        return "%s_%d" % (p, self.n_uid)

    def begin_phase(self):
        self.phase = contextlib.ExitStack()

    def end_phase(self):
        self.barrier()
        self.phase.close()
        self.phase = None

    def sb(self, name, shape, dtype, const=False, perm=False):
        st = self.es if perm else self.phase
        t = st.enter_context(self.nc.sbuf_tensor(self.uid(name), list(shape), dtype))
        return Buf(t, const=const)

    def dram(self, name, shape, dtype, kind=None):
        if kind is None:
            t = self.nc.dram_tensor(name, list(shape), dtype)
        else:
            t = self.nc.dram_tensor(name, list(shape), dtype, kind=kind)
        return Buf(t)

    def barrier(self):
        tks = []
        for E in self.engs:
            if E.seq > 0:
                tks.append(('e', E, E.seq))
        for q in (self.qs, self.qg, self.qs2):
            for s, v in zip(q.sems, q.vals):
                if v > 0:
                    tks.append(('d', s, v))
        for E in self.engs:
            E.wait([t for t in tks if not (t[0] == 'e' and t[1] is E)])

    def V(self, fn, r=(), w=()):
        return self.dve.op(fn, r, w)

    def A(self, fn, r=(), w=()):
        return self.act.op(fn, r, w)

    def T(self, fn, r=(), w=()):
        return self.pe.op(fn, r, w)


class Ring:
    def __init__(self, K, n, elems=4096):
        self.K = K
        self.slots = [K.sb("ring", [128, elems], BF16) for _ in range(n)]
        self.i = 0

    def get(self, src_ap, shape, srcbuf):
        K = self.K
        s = self.slots[self.i % len(self.slots)]
        self.i += 1
        n = int(np.prod(shape[1:]))
        v = s.ap[:, 0:n]
        if len(shape) == 3:
            v = v.rearrange("p (a b) -> p a b", a=shape[1])
        K.qg.dma(out=v, in_=src_ap, r=[srcbuf], w=[s])
        return s, v


def bf16v(psbuf):
    return psbuf.ap[:, :].bitcast(BF16)


class Ctx:
    pass


def setup_consts(K, C, io):
    nc = K.nc
    C.ident = K.sb("ident", [128, 128], BF16, const=True, perm=True)
    K.qg.dma(out=C.ident.ap[:, :], in_=io.ident.ap[:, :], r=[io.ident], w=[C.ident])
    C.eps = K.sb("eps", [128, 1], F32, const=True, perm=True)
    K.V(lambda: nc.vector.memset(C.eps.ap[:, :], EPS), w=[C.eps])
    C.mcol = K.sb("mcol", [128, 1], F32, const=True, perm=True)
    K.qs.dma(out=C.mcol.ap[:, :], in_=io.halo_m.ap[:, :], r=[io.halo_m], w=[C.mcol])
    C.cw = K.sb("cw", [128, 96], F32, const=True, perm=True)
    K.qs.dma(out=C.cw.ap[:, :], in_=io.convw.ap[:, :], r=[io.convw], w=[C.cw])


def norm_block(K, C, xsrc, row0, ntok, g_b, hn, xs, hnT, hnT_parts, st, tb):
    nc = K.nc
    nsub = (ntok + 127) // 128
    xs_l = xs if isinstance(xs, list) else [xs]
    hn_l = hn if isinstance(hn, list) else [hn]
    st_l = st if isinstance(st, list) else [st]
    for ts in range(nsub):
        xs, hn, st = xs_l[ts % len(xs_l)], hn_l[ts % len(hn_l)], st_l[ts % len(st_l)]
        rows = min(128, ntok - ts * 128)
        if rows < 128:
            K.V(lambda: nc.vector.memset(xs.ap[:, :], 0.0), w=[xs])
        K.qs.dma(out=xs.ap[0:rows, :], in_=xsrc.ap[row0 + ts * 128: row0 + ts * 128 + rows, :],
                 r=[xsrc], w=[xs])
        K.A(lambda: nc.scalar.activation(out=hn.ap[:, :], in_=xs.ap[:, :], func=AF.Square,
                                         accum_out=st.ap[:, 0:1]), r=[xs], w=[hn, st])
        K.A(lambda: nc.scalar.activation(out=st.ap[:, 1:2], in_=st.ap[:, 0:1], func=AF.Sqrt,
                                         scale=1.0 / D, bias=C.eps.ap[:, 0:1]), r=[st, C.eps], w=[st])
        K.V(lambda: nc.vector.reciprocal(out=st.ap[:, 2:3], in_=st.ap[:, 1:2]), r=[st], w=[st])
        K.V(lambda: nc.vector.scalar_tensor_tensor(out=hn.ap[:, :], in0=xs.ap[:, :], scalar=st.ap[:, 2:3],
                                                   in1=g_b.ap[:, :], op0=ALU.mult, op1=ALU.mult),
            r=[xs, st, g_b], w=[hn])
        for half in range(2):
            pb = tb[half]
            pv = bf16v(pb)
            for k in range(8):
                kc = half * 8 + k
                K.T(lambda: nc.tensor.transpose(pv[:, k * 128:(k + 1) * 128], hn.ap[:, kc * 128:(kc + 1) * 128],
                                                C.ident.ap[:, :]), r=[hn, C.ident], w=[pb])
            dst = hnT.ap[:, half * 8:(half + 1) * 8, ts * 128:(ts + 1) * 128]
            src = pv.rearrange("p (k t) -> p k t", k=8)
            part = hnT_parts[ts * 2 + half]
            if half == 0:
                K.A(lambda: nc.scalar.copy(out=dst, in_=src), r=[pb], w=[part])
            else:
                K.V(lambda: nc.vector.tensor_copy(out=dst, in_=src), r=[pb], w=[part])


def load_gb(K, g_b, gsrc, row):
    K.qs.dma(out=g_b.ap[:, :], in_=gsrc.ap[row:row + 1, :].partition_broadcast(128), r=[gsrc], w=[g_b])


def mlp_half(K, C, io, li, xin, xout, after_store=None):
    nc = K.nc
    MB = 1024
    NS = MB // 128
    K.begin_phase()
    ring = Ring(K, 10, 2048)
    g_b = K.sb("g_b", [128, D], F32)
    hn = [K.sb("hn", [128, D], BF16) for _ in range(2)]
    xs = [K.sb("xs", [128, D], F32) for _ in range(2)]
    st = [K.sb("st", [128, 4], F32) for _ in range(2)]
    hnT = K.sb("hnT", [128, KC, MB], BF16)
    parts = [Buf(hnT.ap) for _ in range(2 * NS)]
    xacc = K.sb("xacc", [128, NS, D], F32)
    xparts = [[Buf(xacc.ap) for _ in range(4)] for _ in range(NS)]
    xflat = [b for row in xparts for b in row]
    a2T = K.sb("a2T", [128, 2 * G, MB], BF16)
    a2p = [Buf(a2T.ap) for _ in range(2 * G)]
    rt = [K.sb("rt", [128, 512], F32) for _ in range(2)]
    load_gb(K, g_b, io.mlp_g, li)
    nmm = 0
    nrt = 0
    for blk in range(TOK // MB):
        r0 = blk * MB
        for sb_ in range(MB // 512):
            norm_block(K, C, xin, r0 + sb_ * 512, 512, g_b, hn, xs,
                       Buf(hnT.ap[:, :, sb_ * 512:(sb_ + 1) * 512]), parts[sb_ * 8:(sb_ + 1) * 8], st, [K.ps[6], K.ps[7]])
        K.qs.dma(out=xacc.ap[:, :, :], in_=xin.ap[r0:r0 + MB, :].rearrange("(t p) d -> p t d", p=128),
                 r=[xin], w=xflat)
        for g in range(NFC // G):
            w2v = []
            for fi in range(G):
                fc = g * G + fi
                sidx = (g % 2) * G + fi
                slot = a2p[sidx]
                ws, wv = ring.get(io.w1.ap[li, fc], [128, KC, 128], io.w1)
                for th in range(MB // 512):
                    pb = K.ps[nmm % 3]
                    nmm += 1
                    for kc in range(KC):
                        K.T(lambda: nc.tensor.matmul(pb.ap[:, :], wv[:, kc, :], hnT.ap[:, kc, th * 512:(th + 1) * 512],
                                                     start=(kc == 0), stop=(kc == KC - 1)),
                            r=[ws] + parts, w=[pb])
                    rb = rt[nrt % 2]
                    nrt += 1
                    K.A(lambda: nc.scalar.activation(out=rb.ap[:, :], in_=pb.ap[:, :], func=AF.Relu), r=[pb], w=[rb])
                    K.V(lambda: nc.vector.tensor_tensor(out=a2T.ap[:, sidx, th * 512:(th + 1) * 512], in0=rb.ap[:, :],
                                                        in1=rb.ap[:, :], op=ALU.mult), r=[rb], w=[slot])
            for fi in range(G):
                fc = g * G + fi
                w2v.append(ring.get(io.w2.ap[li, fc], [128, D], io.w2))
            for ts in range(NS):
                for dc in range(4):
                    pb = K.ps[3 + (ts * 4 + dc) % 3]
                    for fi in range(G):
                        sidx = (g % 2) * G + fi
                        K.T(lambda: nc.tensor.matmul(pb.ap[:, :], a2T.ap[:, sidx, ts * 128:(ts + 1) * 128],
                                                     w2v[fi][1][:, dc * 512:(dc + 1) * 512],
                                                     start=(fi == 0), stop=(fi == G - 1)),
                            r=[a2p[sidx], w2v[fi][0]], w=[pb])
                    xp = xparts[ts][dc]
                    K.V(lambda: nc.vector.tensor_tensor(out=xacc.ap[:, ts, dc * 512:(dc + 1) * 512],
                                                        in0=pb.ap[:, :],
                                                        in1=xacc.ap[:, ts, dc * 512:(dc + 1) * 512], op=ALU.add),
                        r=[pb], w=[xp])
        K.qs2.dma(out=xout.ap[r0:r0 + MB, :].rearrange("(t p) d -> p t d", p=128), in_=xacc.ap[:, :, :],
                  r=xflat, w=[xout])
        if after_store is not None:
            for b5 in range(MB // 512):
                after_store(blk * (MB // 512) + b5)
    K.end_phase()


def conv_half(K, C, io, cj, xin, xhalo, xout):
    nc = K.nc
    K.begin_phase()
    ring = Ring(K, 8, 4096)
    g_b = K.sb("g_b", [128, D], F32)
    hn = [K.sb("hn", [128, D], BF16) for _ in range(2)]
    xs = [K.sb("xs", [128, D], F32) for _ in range(2)]
    st = [K.sb("st", [128, 4], F32) for _ in range(2)]
    hnT = K.sb("hnT", [128, KC, BLK], BF16)
    parts = [Buf(hnT.ap) for _ in range(8)]
    hnTh = K.sb("hnTh", [128, KC, 128], BF16)
    hparts = [Buf(hnTh.ap) for _ in range(8)]
    xacc = K.sb("xacc", [128, 4, D], F32)
    xparts = [[Buf(xacc.ap) for _ in range(8)] for _ in range(4)]
    xflat = [b for row in xparts for b in row]
    gyT = K.sb("gyT", [128, KC, BLK], BF16)
    gyp = [Buf(gyT.ap) for _ in range(KC)]
    zsave = K.sb("zsave", [128, KC, 2], F32)
    zsp = [Buf(zsave.ap) for _ in range(KC)]
    cs = [K.sb("cs", [128, BLK], F32) for _ in range(2)]
    zt = [K.sb("zt", [128, BLK + 2], F32) for _ in range(2)]
    yt = [K.sb("yt", [128, BLK], F32) for _ in range(2)]
    ch = K.sb("ch", [128, 2], F32)
    load_gb(K, g_b, io.conv_g, cj)
    norm_block(K, C, xhalo, 0, 2, g_b, hn, xs, hnTh, hparts, st, [K.ps[6], K.ps[7]])
    it = 0
    for blk in range(NB):
        r0 = blk * BLK
        norm_block(K, C, xin, r0, BLK, g_b, hn, xs, hnT, parts, st, [K.ps[6], K.ps[7]])
        K.qs.dma(out=xacc.ap[:, :, :], in_=xin.ap[r0:r0 + BLK, :].rearrange("(t p) d -> p t d", p=128),
                 r=[xin], w=xflat)
        for dc in range(KC):
            wb = ring.get(io.cwin.ap[cj, dc], [128, KC, 128], io.cwin)
            wc = ring.get(io.cwin.ap[cj, 16 + dc], [128, KC, 128], io.cwin)
            wu = ring.get(io.cwin.ap[cj, 32 + dc], [128, KC, 128], io.cwin)
            psc, psu, psb, psh = K.ps[(it * 3) % 6], K.ps[(it * 3 + 1) % 6], K.ps[(it * 3 + 2) % 6], K.ps[6 + it % 2]
            csb, ztb, ytb = cs[it % 2], zt[it % 2], yt[it % 2]
            it += 1
            for (wt, pb) in ((wc, psc), (wu, psu), (wb, psb)):
                for kc in range(KC):
                    K.T(lambda: nc.tensor.matmul(pb.ap[:, :], wt[1][:, kc, :], hnT.ap[:, kc, :],
                                                 start=(kc == 0), stop=(kc == KC - 1)),
                        r=[wt[0]] + parts, w=[pb])
            if blk == 0:
                for j, wt in enumerate((wc, wu)):
                    for kc in range(KC):
                        K.T(lambda: nc.tensor.matmul(psh.ap[:, j * 2:j * 2 + 2], wt[1][:, kc, :], hnTh.ap[:, kc, 0:2],
                                                     start=(kc == 0), stop=(kc == KC - 1)),
                            r=[wt[0]] + hparts, w=[psh])
            K.A(lambda: nc.scalar.copy(out=csb.ap[:, :], in_=psc.ap[:, :]), r=[psc], w=[csb])
            K.V(lambda: nc.vector.tensor_tensor(out=ztb.ap[:, 2:BLK + 2], in0=psu.ap[:, :], in1=csb.ap[:, :],
                                                op=ALU.mult), r=[psu, csb], w=[ztb])
            if blk == 0:
                K.A(lambda: nc.scalar.copy(out=ch.ap[:, :], in_=psh.ap[:, 0:2]), r=[psh], w=[ch])
                K.V(lambda: nc.vector.tensor_tensor(out=ztb.ap[:, 0:2], in0=psh.ap[:, 2:4], in1=ch.ap[:, :],
                                                    op=ALU.mult), r=[psh, ch], w=[ztb])
                K.V(lambda: nc.vector.tensor_scalar(out=ztb.ap[:, 0:2], in0=ztb.ap[:, 0:2], scalar1=C.mcol.ap[:, 0:1],
                                                    scalar2=None, op0=ALU.mult), r=[C.mcol], w=[ztb])
            else:
                K.V(lambda: nc.vector.tensor_copy(out=ztb.ap[:, 0:2], in_=zsave.ap[:, dc, :]), r=[zsp[dc]], w=[ztb])
            K.V(lambda: nc.vector.tensor_copy(out=zsave.ap[:, dc, :], in_=ztb.ap[:, BLK:BLK + 2]), r=[ztb], w=[zsp[dc]])
            base = cj * 48
            K.V(lambda: nc.vector.tensor_scalar(out=ytb.ap[:, :], in0=ztb.ap[:, 2:BLK + 2],
                                                scalar1=C.cw.ap[:, base + 32 + dc:base + 33 + dc], scalar2=None,
                                                op0=ALU.mult), r=[ztb, C.cw], w=[ytb])
            K.V(lambda: nc.vector.scalar_tensor_tensor(out=ytb.ap[:, :], in0=ztb.ap[:, 1:BLK + 1],
                                                       scalar=C.cw.ap[:, base + 16 + dc:base + 17 + dc],
                                                       in1=ytb.ap[:, :], op0=ALU.mult, op1=ALU.add),
                r=[ztb, C.cw], w=[ytb])
            K.V(lambda: nc.vector.scalar_tensor_tensor(out=ytb.ap[:, :], in0=ztb.ap[:, 0:BLK],
                                                       scalar=C.cw.ap[:, base + dc:base + dc + 1],
                                                       in1=ytb.ap[:, :], op0=ALU.mult, op1=ALU.add),
                r=[ztb, C.cw], w=[ytb])
            K.V(lambda: nc.vector.tensor_tensor(out=gyT.ap[:, dc, :], in0=psb.ap[:, :], in1=ytb.ap[:, :],
                                                op=ALU.mult), r=[psb, ytb], w=[gyp[dc]])
        for dcol in range(8):
            wo = ring.get(io.cwout.ap[cj, dcol], [128, KC, 256], io.cwout)
            for ts in range(4):
                pb = K.ps[(dcol * 4 + ts) % 6]
                for kc in range(KC):
                    K.T(lambda: nc.tensor.matmul(pb.ap[:, 0:256], gyT.ap[:, kc, ts * 128:(ts + 1) * 128], wo[1][:, kc, :],
                                                 start=(kc == 0), stop=(kc == KC - 1)),
                        r=[wo[0]] + gyp, w=[pb])
                K.V(lambda: nc.vector.tensor_tensor(out=xacc.ap[:, ts, dcol * 256:(dcol + 1) * 256],
                                                    in0=pb.ap[:, 0:256],
                                                    in1=xacc.ap[:, ts, dcol * 256:(dcol + 1) * 256], op=ALU.add),
                    r=[pb], w=[xparts[ts][dcol]])
        K.qs2.dma(out=xout.ap[r0:r0 + BLK, :].rearrange("(t p) d -> p t d", p=128), in_=xacc.ap[:, :, :],
                  r=xflat, w=[xout])
    K.end_phase()


def rope_apply(K, src, dst, nh, hd, R, cosb, sinb, tabs, scr, srcbuf, dstbuf):
    nc = K.nc
    s3 = src.rearrange("p (h d) -> p h d", h=nh)
    d3 = dst.rearrange("p (h d) -> p h d", h=nh)
    t = [scr.ap[:, i * 64:i * 64 + nh * R].rearrange("p (h r) -> p h r", h=nh) for i in range(4)]
    x1 = s3[:, :, 0:R]
    x2 = s3[:, :, R:2 * R]
    K.V(lambda: nc.vector.tensor_tensor(out=t[0], in0=x1, in1=cosb, op=ALU.mult), r=[srcbuf, tabs], w=[scr])
    K.V(lambda: nc.vector.tensor_tensor(out=t[1], in0=x2, in1=sinb, op=ALU.mult), r=[srcbuf, tabs], w=[scr])
    K.V(lambda: nc.vector.tensor_tensor(out=t[2], in0=x2, in1=cosb, op=ALU.mult), r=[srcbuf, tabs], w=[scr])
    K.V(lambda: nc.vector.tensor_tensor(out=t[3], in0=x1, in1=sinb, op=ALU.mult), r=[srcbuf, tabs], w=[scr])
    K.V(lambda: nc.vector.tensor_tensor(out=d3[:, :, 0:R], in0=t[0], in1=t[1], op=ALU.subtract), r=[scr], w=[dstbuf])
    K.V(lambda: nc.vector.tensor_tensor(out=d3[:, :, R:2 * R], in0=t[2], in1=t[3], op=ALU.add), r=[scr], w=[dstbuf])
    K.A(lambda: nc.scalar.copy(out=d3[:, :, 2 * R:hd], in_=s3[:, :, 2 * R:hd]), r=[srcbuf], w=[dstbuf])


def attn_half(K, C, io, aj, xown, xoth, xout):
    nc = K.nc
    K.begin_phase()
    ring = Ring(K, 2, 4096)
    KT = K.sb("KT", [128, 4, 4096], BF16)
    Vt = K.sb("Vt", [128, 32, 512], BF16)
    kiT = K.sb("kiT", [128, 4096], BF16)
    HA = K.sb("HA", [128, KC, BLK], BF16)
    QT = K.sb("QT", [128, 16, BLK], BF16)
    qiT = K.sb("qiT", [128, 8, BLK], BF16)
    maskT = K.sb("maskT", [128, 32, 256], BF16)
    R1 = K.sb("R1", [128, 4096], F32)
    R2 = K.sb("R2", [128, 4096], BF16)
    R1c = [Buf(R1.ap) for _ in range(8)]
    g_b_ap = R1.ap[:, 2048:4096]
    xs_ap = R1.ap[:, 0:2048]
    ang_ap = R1.ap[:, 0:768].rearrange("p (s f) -> p s f", s=32)
    ang2_ap = R1.ap[:, 1024:1792].rearrange("p (s f) -> p s f", s=32)
    angi_f = R1.ap[:, 2048:2816].rearrange("p (s f) -> p s f", s=32)
    angi_ap = R1.ap[:, 2048:2816].bitcast(I32).rearrange("p (s f) -> p s f", s=32)
    ang_b, ang2_b, angi_b = R1c[0:2], R1c[2:4], R1c[4:6]
    st = K.sb("st", [128, 8], F32)
    tabs = K.sb("tabs", [128, 2, 32, 24], F32, const=False)
    posi = K.sb("posi", [128, 32], I32)
    inv = K.sb("inv", [128, 24], F32)
    qg_b = K.sb("qg_b", [128, 128], F32)
    kg_b = K.sb("kg_b", [128, 128], F32)
    tri = K.sb("tri", [128, 128], F32)
    ones = K.sb("ones", [128, 128], BF16)
    penm = K.sb("penm", [128, 1], F32)
    pj2 = [K.sb("pj", [128, 512], F32) for _ in range(2)]
    pjb2 = [K.sb("pjb", [128, 512], BF16) for _ in range(2)]
    scr2 = [K.sb("scr", [128, 256], F32) for _ in range(2)]
    sq2 = [K.sb("sq", [128, 512], F32) for _ in range(2)]
    st2 = [K.sb("sth", [128, 4], F32) for _ in range(2)]
    wis = K.sb("wis", [128, 4, 16], F32)
    rbuf = [K.sb("rbuf", [128, 512], BF16) for _ in range(4)]
    ebuf = [K.sb("ebuf", [128, 512], BF16) for _ in range(5)]
    pbuf = [K.sb("pbuf", [128, 512], BF16) for _ in range(5)]
    rden2 = [K.sb("rden", [128, 512], F32) for _ in range(2)]
    bis = K.sb("bis", [128, 40], F32)
    PS = K.ps

    K.qs.dma(out=tri.ap[:, :], in_=io.tri.ap[:, :], r=[io.tri], w=[tri])
    K.V(lambda: nc.vector.memset(ones.ap[:, :], 1.0), w=[ones])
    K.V(lambda: nc.vector.tensor_scalar(out=penm.ap[:, :], in0=C.mcol.ap[:, :], scalar1=-1.0, scalar2=BIG,
                                        op0=ALU.add, op1=ALU.mult), r=[C.mcol], w=[penm])
    K.qs.dma(out=qg_b.ap[:, :], in_=io.q_g.ap[aj:aj + 1, :].partition_broadcast(128), r=[io.q_g], w=[qg_b])
    K.qs.dma(out=kg_b.ap[:, :], in_=io.k_g.ap[aj:aj + 1, :].partition_broadcast(128), r=[io.k_g], w=[kg_b])
    K.qs.dma(out=inv.ap[:, :], in_=io.ropeinv.ap[0:1, :].partition_broadcast(128), r=[io.ropeinv], w=[inv])
    K.qs.dma(out=posi.ap[:, :], in_=io.pos.ap[:, :], r=[io.pos], w=[posi])
    posfT = K.sb("posf", [128, 32], F32)
    K.V(lambda: nc.vector.tensor_copy(out=posfT.ap[:, :], in_=posi.ap[:, :]), r=[posi], w=[posfT])
    K.V(lambda: nc.vector.tensor_tensor(out=ang_ap, in0=posfT.ap[:, :].unsqueeze(2).broadcast_to([128, 32, 24]),
                                        in1=inv.ap[:, :].unsqueeze(1).broadcast_to([128, 32, 24]), op=ALU.mult),
        r=[posfT, inv], w=ang_b)
    TWO_PI = 2.0 * np.pi
    for which in range(2):
        if which == 1:
            K.V(lambda: nc.vector.tensor_scalar(out=ang_ap, in0=ang_ap, scalar1=float(np.pi / 2),
                                                scalar2=None, op0=ALU.add), r=ang_b, w=ang_b)
        K.V(lambda: nc.vector.tensor_scalar(out=ang2_ap, in0=ang_ap, scalar1=float(1.0 / TWO_PI),
                                            scalar2=None, op0=ALU.mult), r=ang_b, w=ang2_b)
        K.V(lambda: nc.vector.tensor_copy(out=angi_ap, in_=ang2_ap), r=ang2_b, w=angi_b)
        K.V(lambda: nc.vector.tensor_copy(out=ang2_ap, in_=angi_ap), r=angi_b, w=ang2_b)
        K.V(lambda: nc.vector.scalar_tensor_tensor(out=ang2_ap, in0=ang2_ap, scalar=float(-TWO_PI),
                                                   in1=ang_ap, op0=ALU.mult, op1=ALU.add),
            r=ang2_b + ang_b, w=ang2_b)
        K.V(lambda: nc.vector.tensor_scalar(out=angi_f, in0=ang2_ap, scalar1=float(np.pi),
                                            scalar2=float(-TWO_PI), op0=ALU.is_gt, op1=ALU.mult), r=ang2_b, w=angi_b)
        K.V(lambda: nc.vector.tensor_tensor(out=ang2_ap, in0=ang2_ap, in1=angi_f,
                                            op=ALU.add), r=ang2_b + angi_b, w=ang2_b)
        K.V(lambda: nc.vector.tensor_scalar(out=angi_f, in0=ang2_ap, scalar1=float(-np.pi),
                                            scalar2=float(TWO_PI), op0=ALU.is_lt, op1=ALU.mult), r=ang2_b, w=angi_b)
        K.V(lambda: nc.vector.tensor_tensor(out=ang2_ap, in0=ang2_ap, in1=angi_f,
                                            op=ALU.add), r=ang2_b + angi_b, w=ang2_b)
        K.V(lambda: nc.vector.tensor_scalar(out=ang2_ap, in0=ang2_ap, scalar1=float(np.pi - 1e-6),
                                            scalar2=float(-np.pi + 1e-6), op0=ALU.min, op1=ALU.max), r=ang2_b, w=ang2_b)
        K.A(lambda: nc.scalar.activation(out=tabs.ap[:, which, :, :], in_=ang2_ap, func=AF.Sin),
            r=ang2_b, w=[tabs])

    def tab(which, sub, off, R, nh):
        return tabs.ap[:, which, sub, off:off + R].unsqueeze(1).broadcast_to([128, nh, R])

    ntp = [0]

    def transpose_to(src_ap, srcbuf, dst_ap, dstbuf, nrows=128):
        pb = PS[2 + ntp[0] % 2]
        ntp[0] += 1
        pv = bf16v(pb)
        K.T(lambda: nc.tensor.transpose(pv[0:nrows, 0:128], src_ap, C.ident.ap[:, :]), r=[srcbuf, C.ident], w=[pb])
        K.A(lambda: nc.scalar.copy(out=dst_ap, in_=pv[0:nrows, 0:128]), r=[pb], w=[dstbuf])

    nproj = [0]
    xg = {}

    def exchange_send():
        xg['k'] = []
        xg['v'] = []
        for i in range(2):
            kd = K.dram(K.uid("kx"), [128, 4096], BF16)
            kg = K.dram(K.uid("kxg"), [256, 4096], BF16)
            K.qs.dma(out=kd.ap[:, :].rearrange("p (h t) -> p h t", h=2), in_=KT.ap[:, 2 * i:2 * i + 2, 2048:4096],
                     r=[KT], w=[kd])
            allgather(K, kd, kg)
            xg['k'].append(kg)
            vd = K.dram(K.uid("vx"), [128, 4096], BF16)
            vg = K.dram(K.uid("vxg"), [256, 4096], BF16)
            K.qs.dma(out=vd.ap[:, :].rearrange("p (k c) -> p k c", k=8), in_=Vt.ap[:, 16 + 8 * i:24 + 8 * i, :],
                     r=[Vt], w=[vd])
            allgather(K, vd, vg)
            xg['v'].append(vg)
        idr = K.dram(K.uid("ix"), [128, 2048], BF16)
        ig = K.dram(K.uid("ixg"), [256, 2048], BF16)
        K.qs.dma(out=idr.ap[:, :], in_=kiT.ap[:, 2048:4096], r=[kiT], w=[idr])
        allgather(K, idr, ig)
        xg['i'] = ig

    def exchange_recv():
        for i in range(2):
            K.qs.dma(out=KT.ap[:, 2 * i:2 * i + 2, 0:2048],
                     in_=xg['k'][i].ap[0:128, :].rearrange("p (h t) -> p h t", h=2), r=[xg['k'][i]], w=[KT])
            K.qs.dma(out=Vt.ap[:, 8 * i:8 * i + 8, :],
                     in_=xg['v'][i].ap[0:128, :].rearrange("p (k c) -> p k c", k=8), r=[xg['v'][i]], w=[Vt])
        K.qs.dma(out=kiT.ap[:, 0:2048], in_=xg['i'].ap[0:128, :], r=[xg['i']], w=[kiT])
    nix = [0]
    nout = [0]
    QTs = [Buf(QT.ap) for _ in range(4)]
    gsrc = io.attn_g

    for blk8 in range(8):
        ph = blk8 // 4
        own = True
        blk = blk8 % 4
        r0 = blk * BLK
        K.qs.dma(out=g_b_ap, in_=gsrc.ap[aj:aj + 1, :].partition_broadcast(128), r=[gsrc], w=R1c[4:8])
        nsub = 4
        for ts in range(nsub):
            if own:
                xsb, xsa = xown, xown.ap[r0 + ts * 128: r0 + (ts + 1) * 128, :]
            else:
                xsb, xsa = xoth(r0 + ts * 128)
            K.qs.dma(out=xs_ap, in_=xsa, r=[xsb], w=R1c[0:4])
            K.A(lambda: nc.scalar.activation(out=R2.ap[:, 0:2048], in_=xs_ap, func=AF.Square,
                                             accum_out=st.ap[:, 0:1]), r=R1c[0:4], w=[R2, st])
            K.A(lambda: nc.scalar.activation(out=st.ap[:, 1:2], in_=st.ap[:, 0:1], func=AF.Sqrt,
                                             scale=1.0 / D, bias=C.eps.ap[:, 0:1]), r=[st, C.eps], w=[st])
            K.V(lambda: nc.vector.reciprocal(out=st.ap[:, 2:3], in_=st.ap[:, 1:2]), r=[st], w=[st])
            K.V(lambda: nc.vector.scalar_tensor_tensor(out=R2.ap[:, 0:2048], in0=xs_ap, scalar=st.ap[:, 2:3],
                                                       in1=g_b_ap, op0=ALU.mult, op1=ALU.mult),
                r=R1c + [st], w=[R2])
            for half in range(2):
                pb = PS[2 + half]
                pv = bf16v(pb)
                for k in range(8):
                    kc = half * 8 + k
                    K.T(lambda: nc.tensor.transpose(pv[:, k * 128:(k + 1) * 128], R2.ap[:, kc * 128:(kc + 1) * 128],
                                                    C.ident.ap[:, :]), r=[R2, C.ident], w=[pb])
                dst = HA.ap[:, half * 8:(half + 1) * 8, ts * 128:(ts + 1) * 128]
                src = pv.rearrange("p (k t) -> p k t", k=8)
                if half == 0:
                    K.A(lambda: nc.scalar.copy(out=dst, in_=src), r=[pb], w=[HA])
                else:
                    K.V(lambda: nc.vector.tensor_copy(out=dst, in_=src), r=[pb], w=[HA])
        sub0 = (16 if own else 0) + blk * 4
        tok0 = (2048 if own else 0) + r0

        def run_proj(wap, ncol, tp):
            ws, wv = wap
            pb = PS[nproj[0] % 2]
            nproj[0] += 1
            for t2 in range(2):
                ts = 2 * tp + t2
                for kc in range(KC):
                    K.T(lambda: nc.tensor.matmul(pb.ap[:, t2 * ncol:(t2 + 1) * ncol], HA.ap[:, kc, ts * 128:(ts + 1) * 128],
                                                 wv[:, kc, :], start=(kc == 0), stop=(kc == KC - 1)), r=[ws, HA], w=[pb])
            par = nproj[0] % 2
            return pb, pj2[par], pjb2[par], scr2[par], sq2[par], st2[par]

        def headnorm(pb, gbuf, pj, sq, sth):
            pb3 = pb.ap[:, 0:512].rearrange("p (g d) -> p g d", g=4)
            pj3 = pj.ap[:, 0:512].rearrange("p (g d) -> p g d", g=4)
            K.A(lambda: nc.scalar.activation(out=sq.ap[:, 0:512], in_=pb.ap[:, 0:512], func=AF.Square), r=[pb], w=[sq])
            K.V(lambda: nc.vector.tensor_reduce(out=sth.ap[:, 0:4], in_=sq.ap[:, 0:512].rearrange("p (g d) -> p g d", g=4),
                                                axis=AX.X, op=ALU.add), r=[sq], w=[sth])
            K.A(lambda: nc.scalar.activation(out=sth.ap[:, 0:4], in_=sth.ap[:, 0:4], func=AF.Sqrt,
                                             scale=1.0 / 128, bias=C.eps.ap[:, 0:1]), r=[sth, C.eps], w=[sth])
            K.V(lambda: nc.vector.reciprocal(out=sth.ap[:, 0:4], in_=sth.ap[:, 0:4]), r=[sth], w=[sth])
            K.V(lambda: nc.vector.tensor_tensor(out=pj3, in0=pb3, in1=sth.ap[:, 0:4].unsqueeze(2).broadcast_to([128, 4, 128]),
                                                op=ALU.mult), r=[pb, sth], w=[pj])
            K.V(lambda: nc.vector.tensor_tensor(out=pj3, in0=pj3, in1=gbuf.ap[:, :].unsqueeze(1).broadcast_to([128, 4, 128]),
                                                op=ALU.mult), r=[gbuf], w=[pj])

        def rope2(pj, pjb, scr, nh, hd, R, off, sub):
            n = 2 * nh * hd
            s4 = pj.ap[:, 0:n].rearrange("p (t h d) -> p t h d", t=2, h=nh)
            d4 = pjb.ap[:, 0:n].rearrange("p (t h d) -> p t h d", t=2, h=nh)
            cosb = tabs.ap[:, 1, sub:sub + 2, off:off + R].unsqueeze(2).broadcast_to([128, 2, nh, R])
            sinb = tabs.ap[:, 0, sub:sub + 2, off:off + R].unsqueeze(2).broadcast_to([128, 2, nh, R])
            t = [scr.ap[:, i * 64:i * 64 + 2 * nh * R].rearrange("p (t h r) -> p t h r", t=2, h=nh) for i in range(4)]
            x1 = s4[:, :, :, 0:R]
            x2 = s4[:, :, :, R:2 * R]
            K.V(lambda: nc.vector.tensor_tensor(out=t[0], in0=x1, in1=cosb, op=ALU.mult), r=[pj, tabs], w=[scr])
            K.V(lambda: nc.vector.tensor_tensor(out=t[1], in0=x2, in1=sinb, op=ALU.mult), r=[pj, tabs], w=[scr])
            K.V(lambda: nc.vector.tensor_tensor(out=t[2], in0=x2, in1=cosb, op=ALU.mult), r=[pj, tabs], w=[scr])
            K.V(lambda: nc.vector.tensor_tensor(out=t[3], in0=x1, in1=sinb, op=ALU.mult), r=[pj, tabs], w=[scr])
            K.V(lambda: nc.vector.tensor_tensor(out=d4[:, :, :, 0:R], in0=t[0], in1=t[1], op=ALU.subtract), r=[scr], w=[pjb])
            K.V(lambda: nc.vector.tensor_tensor(out=d4[:, :, :, R:2 * R], in0=t[2], in1=t[3], op=ALU.add), r=[scr], w=[pjb])
            K.A(lambda: nc.scalar.copy(out=d4[:, :, :, 2 * R:hd], in_=s4[:, :, :, 2 * R:hd]), r=[pj], w=[pjb])

        def transpose2(pjb, ncol, c0, dst_ap, dstbuf):
            pb = PS[2 + ntp[0] % 2]
            ntp[0] += 1
            pv = bf16v(pb)
            for t2 in range(2):
                K.T(lambda: nc.tensor.transpose(pv[:, t2 * 128:(t2 + 1) * 128],
                                                pjb.ap[:, t2 * ncol + c0:t2 * ncol + c0 + 128], C.ident.ap[:, :]),
                    r=[pjb, C.ident], w=[pb])
            K.A(lambda: nc.scalar.copy(out=dst_ap, in_=pv[:, 0:256]), r=[pb], w=[dstbuf])

        def do_kv():
            for cb in range(2):
                wk = ring.get(io.wkv.ap[aj, cb], [128, KC, 256], io.wkv)
                for tp in range(2):
                    pb, pj, pjb, scr, sq, sth = run_proj(wk, 256, tp)
                    headnorm(pb, kg_b, pj, sq, sth)
                    rope2(pj, pjb, scr, 2, 128, 16, 0, sub0 + 2 * tp)
                    for h in range(2):
                        transpose2(pjb, 256, h * 128, KT.ap[:, cb * 2 + h, tok0 + tp * 256: tok0 + (tp + 1) * 256], KT)
            for cb in range(2):
                wv_ = ring.get(io.wkv.ap[aj, 2 + cb], [128, KC, 256], io.wkv)
                for tp in range(2):
                    pb = run_proj(wv_, 256, tp)[0]
                    kt = tok0 // 128 + 2 * tp
                    K.A(lambda: nc.scalar.copy(out=Vt.ap[:, kt:kt + 2, cb * 256:(cb + 1) * 256],
                                               in_=pb.ap[:, 0:512].rearrange("p (t c) -> p t c", t=2)), r=[pb], w=[Vt])
            wki = ring.get(io.wki.ap[aj], [128, KC, 128], io.wki)
            for tp in range(2):
                pb, pj, pjb, scr, sq, sth = run_proj(wki, 128, tp)
                K.A(lambda: nc.scalar.copy(out=pj.ap[:, 0:256], in_=pb.ap[:, 0:256]), r=[pb], w=[pj])
                rope2(pj, pjb, scr, 2, 64, 8, 16, sub0 + 2 * tp)
                transpose2(pjb, 128, 0, kiT.ap[:, tok0 + tp * 256: tok0 + (tp + 1) * 256], kiT)
        if ph == 0:
            do_kv()
            if blk == 3:
                exchange_send()
            continue
        for cb in range(8):
            wq = ring.get(io.wq.ap[aj, cb], [128, KC, 256], io.wq)
            for tp in range(2):
                pb, pj, pjb, scr, sq, sth = run_proj(wq, 256, tp)
                headnorm(pb, qg_b, pj, sq, sth)
                rope2(pj, pjb, scr, 2, 128, 16, 0, sub0 + 2 * tp)
                for h in range(2):
                    transpose2(pjb, 256, h * 128, QT.ap[:, cb * 2 + h, tp * 256:(tp + 1) * 256], QT)
        for cb in range(4):
            wqi = ring.get(io.wqi.ap[aj, cb], [128, KC, 256], io.wqi)
            for tp in range(2):
                pb, pj, pjb, scr, sq, sth = run_proj(wqi, 256, tp)
                K.A(lambda: nc.scalar.copy(out=pj.ap[:, 0:512], in_=pb.ap[:, 0:512]), r=[pb], w=[pj])
                rope2(pj, pjb, scr, 4, 64, 8, 16, sub0 + 2 * tp)
                for h2 in range(2):
                    transpose2(pjb, 256, h2 * 128, qiT.ap[:, cb * 2 + h2, tp * 256:(tp + 1) * 256], qiT)
        wwi = ring.get(io.wwi.ap[aj], [128, KC, 16], io.wwi)
        for tp in range(2):
            pb = run_proj(wwi, 16, tp)[0]
            K.V(lambda: nc.vector.tensor_scalar(out=wis.ap[:, 2 * tp:2 * tp + 2, :],
                                                in0=pb.ap[:, 0:32].rearrange("p (t c) -> p t c", t=2), scalar1=1.0 / 32.0,
                                                scalar2=None, op0=ALU.mult), r=[pb], w=[wis])

        if blk == 0:
            exchange_recv()
        for qh in range(2):
            nkt = 16 + blk * 4 + qh * 2 + 2
            for jj in range(2):
                j = qh * 2 + jj
                jt = blk * 4 + j
                NK = 2048 + (jt + 1) * 128
                sc = R1
                nch = (NK + 511) // 512
                scr_ = R1c[0:nch]
                IB = [PS[0], PS[1], PS[4], PS[5]]
                for h in range(16):
                    lo = (h % 2) * 64
                    for c in range(nch):
                        cw_ = min(512, NK - c * 512)
                        pb = IB[nix[0] % 4]
                        rb = rbuf[nix[0] % 4]
                        nix[0] += 1
                        K.T(lambda: nc.tensor.matmul(pb.ap[:, 0:cw_], qiT.ap[lo:lo + 64, h // 2, j * 128:(j + 1) * 128],
                                                     kiT.ap[lo:lo + 64, c * 512:c * 512 + cw_], start=True, stop=True),
                            r=[qiT, kiT], w=[pb])
                        K.A(lambda: nc.scalar.activation(out=rb.ap[:, 0:cw_], in_=pb.ap[:, 0:cw_], func=AF.Relu),
                            r=[pb], w=[rb])
                        if h == 0:
                            K.V(lambda: nc.vector.tensor_scalar(out=sc.ap[:, c * 512:c * 512 + cw_], in0=rb.ap[:, 0:cw_],
                                                                scalar1=wis.ap[:, j, 0:1], scalar2=None, op0=ALU.mult),
                                r=[rb, wis], w=[R1c[c]])
                        else:
                            K.V(lambda: nc.vector.scalar_tensor_tensor(out=sc.ap[:, c * 512:c * 512 + cw_],
                                                                       in0=rb.ap[:, 0:cw_], scalar=wis.ap[:, j, h:h + 1],
                                                                       in1=sc.ap[:, c * 512:c * 512 + cw_],
                                                                       op0=ALU.mult, op1=ALU.add),
                                r=[rb, wis], w=[R1c[c]])
                K.V(lambda: nc.vector.tensor_reduce(out=bis.ap[:, 0:1], in_=sc.ap[:, 0:NK], axis=AX.X, op=ALU.min),
                    r=scr_, w=[bis])
                K.V(lambda: nc.vector.tensor_scalar(out=sc.ap[:, 0:2048], in0=sc.ap[:, 0:2048], scalar1=penm.ap[:, 0:1],
                                                    scalar2=None, op0=ALU.add), r=[penm], w=R1c[0:4])
                K.V(lambda: nc.vector.tensor_tensor(out=sc.ap[:, NK - 128:NK], in0=sc.ap[:, NK - 128:NK], in1=tri.ap[:, :],
                                                    op=ALU.add), r=[tri], w=[R1c[(NK - 128) // 512]])
                K.V(lambda: nc.vector.tensor_reduce(out=bis.ap[:, 1:2], in_=sc.ap[:, 0:NK], axis=AX.X, op=ALU.max),
                    r=scr_, w=[bis])
                K.V(lambda: nc.vector.tensor_scalar(out=bis.ap[:, 0:1], in0=bis.ap[:, 0:1], scalar1=-1.0, scalar2=None,
                                                    op0=ALU.add), r=[bis], w=[bis])
                K.V(lambda: nc.vector.scalar_tensor_tensor(out=bis.ap[:, 2:3], in0=bis.ap[:, 0:1], scalar=-1.0,
                                                           in1=bis.ap[:, 1:2], op0=ALU.mult, op1=ALU.add),
                    r=[bis], w=[bis])
                K.V(lambda: nc.vector.tensor_scalar(out=bis.ap[:, 2:3], in0=bis.ap[:, 2:3], scalar1=1e-3, scalar2=None,
                                                    op0=ALU.add), r=[bis], w=[bis])
                for k in range(NBIS + 1):
                    K.V(lambda: nc.vector.tensor_scalar(out=bis.ap[:, 8 + k:9 + k], in0=bis.ap[:, 2:3],
                                                        scalar1=float(2.0 ** -(k + 1)), scalar2=None, op0=ALU.mult),
                        r=[bis], w=[bis])
                K.V(lambda: nc.vector.tensor_tensor(out=bis.ap[:, 3:4], in0=bis.ap[:, 0:1], in1=bis.ap[:, 8:9], op=ALU.add),
                    r=[bis], w=[bis])
                for k in range(NBIS):
                    K.V(lambda: nc.vector.tensor_scalar(out=R2.ap[:, 0:NK], in0=sc.ap[:, 0:NK], scalar1=bis.ap[:, 3:4],
                                                        scalar2=None, op0=ALU.is_ge, op1=ALU.add,
                                                        accum_out=bis.ap[:, 4:5]), r=scr_ + [bis], w=[R2, bis])
                    K.V(lambda: nc.vector.tensor_scalar(out=bis.ap[:, 5:6], in0=bis.ap[:, 4:5], scalar1=TOPK - 0.5,
                                                        scalar2=0.5, op0=ALU.is_ge, op1=ALU.subtract), r=[bis], w=[bis])
                    K.V(lambda: nc.vector.scalar_tensor_tensor(out=bis.ap[:, 3:4], in0=bis.ap[:, 5:6],
                                                               scalar=bis.ap[:, 8 + k:9 + k], in1=bis.ap[:, 3:4],
                                                               op0=ALU.mult, op1=ALU.add), r=[bis], w=[bis])
                K.V(lambda: nc.vector.tensor_tensor(out=bis.ap[:, 6:7], in0=bis.ap[:, 3:4], in1=bis.ap[:, 8 + NBIS:9 + NBIS],
                                                    op=ALU.subtract), r=[bis], w=[bis])
                K.V(lambda: nc.vector.tensor_scalar(out=R2.ap[:, 0:NK], in0=sc.ap[:, 0:NK], scalar1=bis.ap[:, 6:7],
                                                    scalar2=None, op0=ALU.is_ge), r=scr_ + [bis], w=[R2])
                nk_t = NK // 128
                for k0 in range(0, nk_t, 8):
                    kn = min(8, nk_t - k0)
                    pb = PS[2 + (k0 // 8) % 2]
                    pv = bf16v(pb)
                    for k in range(kn):
                        K.T(lambda: nc.tensor.transpose(pv[:, k * 128:(k + 1) * 128],
                                                        R2.ap[:, (k0 + k) * 128:(k0 + k + 1) * 128], C.ident.ap[:, :]),
                            r=[R2, C.ident], w=[pb])
                    K.A(lambda: nc.scalar.copy(out=maskT.ap[:, k0:k0 + kn, jj * 128:(jj + 1) * 128],
                                               in_=pv[:, 0:kn * 128].rearrange("p (k t) -> p k t", k=kn)),
                        r=[pb], w=[maskT])
                if nk_t < nkt:
                    K.V(lambda: nc.vector.memset(maskT.ap[:, nk_t:nkt, jj * 128:(jj + 1) * 128], 0.0), w=[maskT])
            q0 = qh * 256
            units = [(hp, kt) for hp in range(8) for kt in range(nkt)]
            SB_ = [PS[4], PS[5], PS[1], PS[0]]
            LOOK = 3

            def emit_s(u):
                hp, kt = units[u]
                kvh = hp // 2
                psS = SB_[u % 4]
                eb = ebuf[u % 5]
                pbf = pbuf[u % 5]
                K.T(lambda: nc.tensor.matmul(psS.ap[:, 0:512], KT.ap[:, kvh, kt * 128:(kt + 1) * 128],
                                             QT.ap[:, 2 * hp:2 * hp + 2, q0:q0 + 256], start=True, stop=True),
                    r=[KT, QT], w=[psS])
                K.A(lambda: nc.scalar.activation(out=eb.ap[:, :], in_=psS.ap[:, :], func=AF.Exp,
                                                 scale=float(128 ** -0.5)), r=[psS], w=[eb])
                K.V(lambda: nc.vector.tensor_tensor(out=pbf.ap[:, :].rearrange("p (k t) -> p k t", k=2),
                                                    in0=eb.ap[:, :].rearrange("p (k t) -> p k t", k=2),
                                                    in1=maskT.ap[:, kt, :].unsqueeze(1).broadcast_to([128, 2, 256]),
                                                    op=ALU.mult), r=[eb, maskT], w=[pbf])

            def emit_od(u):
                hp, kt = units[u]
                kvh = hp // 2
                pbf = pbuf[u % 5]
                psO, psD = (PS[6], PS[7]) if hp % 2 == 0 else (PS[2], PS[3])
                first = (kt == 0)
                last = (kt == nkt - 1)
                K.T(lambda: nc.tensor.matmul(psO.ap[:, 0:512], Vt.ap[:, kt, kvh * 128:(kvh + 1) * 128],
                                             pbf.ap[:, 0:512], start=first, stop=last), r=[Vt, pbf], w=[psO])
                K.T(lambda: nc.tensor.matmul(psD.ap[:, 0:512], ones.ap[:, :], pbf.ap[:, 0:512],
                                             start=first, stop=last), r=[ones, pbf], w=[psD])
                if last:
                    rden = rden2[hp % 2]
                    K.V(lambda: nc.vector.reciprocal(out=rden.ap[:, :], in_=psD.ap[:, 0:512]), r=[psD], w=[rden])
                    K.V(lambda: nc.vector.tensor_tensor(out=HA.ap[:, 2 * hp:2 * hp + 2, q0:q0 + 256],
                                                        in0=psO.ap[:, 0:512].rearrange("p (k t) -> p k t", k=2),
                                                        in1=rden.ap[:, :].rearrange("p (k t) -> p k t", k=2),
                                                        op=ALU.mult), r=[psO, rden], w=[HA])

            for u in range(len(units) + LOOK):
                if u < len(units):
                    emit_s(u)
                if u >= LOOK:
                    emit_od(u - LOOK)
        xslots = [(QTs[0], QT, QT.ap[:, 0:8, :].rearrange("p a (b c) -> p (a b) c", c=256)),
                  (QTs[1], QT, QT.ap[:, 8:16, :].rearrange("p a (b c) -> p (a b) c", c=256)),
                  (QTs[2], qiT, qiT.ap[:, :, :].rearrange("p a (b c) -> p (a b) c", c=256))]
        for sbq, par_, _v in xslots:
            sbq.w = dict(par_.w)
            sbq.r = dict(par_.r)
        xs2 = R1.ap[:, :].rearrange("p (t d) -> p t d", t=2)
        for tpair in range(2):
            rr = r0 + tpair * 256
            K.qs.dma(out=xs2, in_=xown.ap[rr:rr + 256, :].rearrange("(t p) d -> p t d", p=128), r=[xown], w=R1c)
            for dcol in range(8):
                k6 = nout[0] % 5
                nout[0] += 1
                if k6 < 2:
                    wo = ring.get(io.wao.ap[aj, dcol], [128, KC, 256], io.wao)
                else:
                    sbq, par_, vq = xslots[k6 - 2]
                    K.qg.dma(out=vq, in_=io.wao.ap[aj, dcol], r=[io.wao], w=[sbq])
                    wo = (sbq, vq)
                for t in range(2):
                    ts = 2 * tpair + t
                    pb = PS[(dcol * 2 + t) % 2]
                    for h in range(16):
                        K.T(lambda: nc.tensor.matmul(pb.ap[:, 0:256], HA.ap[:, h, ts * 128:(ts + 1) * 128], wo[1][:, h, :],
                                                     start=(h == 0), stop=(h == 15)), r=[wo[0], HA], w=[pb])
                    c0 = t * 2048 + dcol * 256
                    K.V(lambda: nc.vector.tensor_tensor(out=R1.ap[:, c0:c0 + 256], in0=pb.ap[:, 0:256],
                                                        in1=R1.ap[:, c0:c0 + 256], op=ALU.add),
                        r=[pb], w=[R1c[c0 // 512]])
            K.qs2.dma(out=xout.ap[rr:rr + 256, :].rearrange("(t p) d -> p t d", p=128), in_=xs2, r=R1c, w=[xout])
        for sbq, par_, _v in xslots:
            for tk in list(sbq.w.values()) + list(sbq.r.values()):
                _merge(par_.r, tk)
    K.end_phase()


PAIRS = [[0, 1], [2, 3], [4, 5], [6, 7]]


def allgather(K, src, dst):
    E = K.pool
    sem = K.newsem(K.uid("cc"))
    E.wait(E.deps([src], [dst]))
    ins = K.nc.gpsimd.collective_compute("AllGather", ALU.bypass, replica_groups=PAIRS,
                                         ins=[src.ap.ap().opt()], outs=[dst.ap.ap().opt()])
    ins.then_inc(sem)
    t = ('d', sem, 1)
    _merge(src.r, t)
    dst.w = {}
    dst.r = {}
    _merge(dst.w, t)


def declare_io(K):
    io = Ctx()
    ext = "ExternalInput"
    io.ident = K.dram("ident", [128, 128], F32, ext)
    io.halo_m = K.dram("halo_m", [128, 1], F32, ext)
    io.convw = K.dram("convw", [128, 96], F32, ext)
    io.xown = K.dram("xown", [TOK, D], F32, ext)
    io.xoth = K.dram("xoth", [TOK, D], F32, ext)
    io.mlp_g = K.dram("mlp_g", [4, D], F32, ext)
    io.w1 = K.dram("w1", [4, NFC, 128, KC, 128], F32, ext)
    io.w2 = K.dram("w2", [4, NFC, 128, D], F32, ext)
    io.tri = K.dram("tri", [128, 128], F32, ext)
    io.ropeinv = K.dram("ropeinv", [1, 24], F32, ext)
    io.pos = K.dram("pos", [128, 32], I32, ext)
    io.attn_g = K.dram("attn_g", [2, D], F32, ext)
    io.q_g = K.dram("q_g", [2, 128], F32, ext)
    io.k_g = K.dram("k_g", [2, 128], F32, ext)
    io.wq = K.dram("wq", [2, 8, 128, KC, 256], F32, ext)
    io.wkv = K.dram("wkv", [2, 4, 128, KC, 256], F32, ext)
    io.wki = K.dram("wki", [2, 128, KC, 128], F32, ext)
    io.wqi = K.dram("wqi", [2, 4, 128, KC, 256], F32, ext)
    io.wwi = K.dram("wwi", [2, 128, KC, 16], F32, ext)
    io.wao = K.dram("wao", [2, 8, 128, KC, 256], F32, ext)
    io.conv_g = K.dram("conv_g", [2, D], F32, ext)
    io.cwin = K.dram("cwin", [2, 48, 128, KC, 128], F32, ext)
    io.cwout = K.dram("cwout", [2, 8, 128, KC, 256], F32, ext)
    io.y = K.dram("y", [TOK, D], F32, "ExternalOutput")
    return io


def build_fused():
    K = Kern()
    C = Ctx()
    io = declare_io(K)
    setup_consts(K, C, io)
    xcur = io.xown
    xoth = lambda r: (io.xoth, io.xoth.ap[r:r + 128, :])
    for li in range(4):
        j = li // 2
        xmid = K.dram("xmid%d" % li, [TOK, D], F32)
        if li % 2 == 0:
            attn_half(K, C, io, j, xcur, xoth, xmid)
        else:
            hb = K.dram("hb%d" % li, [2, D], F32)
            hall = K.dram("hall%d" % li, [4, D], F32)
            K.qs.dma(out=hb.ap[:, :], in_=xcur.ap[TOK - 2:TOK, :], r=[xcur], w=[hb])
            allgather(K, hb, hall)
            conv_half(K, C, io, j, xcur, hall, xmid)
        xnext = io.y if li == 3 else K.dram("xr%d" % li, [TOK, D], F32)
        hook = None
        mlp_half(K, C, io, li, xmid, xnext, after_store=hook)
        xcur = xnext
    K.barrier()
    return K.nc


def colblocks(W, bw):
    Cc = W.shape[1]
    return np.ascontiguousarray(W.reshape(KC, 128, Cc // bw, bw).transpose(2, 1, 0, 3))


_PROG = []


def kernel(x, positions, attn_norm_g, attn_w_in, attn_q_norm_g, attn_k_norm_g, attn_w_out,
           conv_norm_g, conv_w_in, conv_w, conv_w_out, mlp_norm_g, mlp_w1, mlp_w2):
    f32 = np.float32
    x = np.asarray(x, f32)
    positions = np.asarray(positions, np.int32)
    ident = np.eye(128, dtype=f32)
    tri = np.where(np.arange(128)[None, :] <= np.arange(128)[:, None], 0.0, -BIG).astype(f32)
    inv_main = (ROPE_THETA ** (-(np.arange(0, 32, 2, dtype=f32)) / f32(32))).astype(f32)
    inv_idx = (ROPE_THETA ** (-(np.arange(0, 16, 2, dtype=f32)) / f32(16))).astype(f32)
    ropeinv = np.concatenate([inv_main, inv_idx])[None, :].astype(f32)
    conv_w = np.asarray(conv_w, f32)
    convw = np.ascontiguousarray(conv_w.reshape(2, 3, 16, 128).transpose(3, 0, 1, 2).reshape(128, 96))
    xc = x.reshape(8, TOK, D)
    w1 = np.stack([colblocks(np.asarray(mlp_w1[l], f32), 128) for l in range(4)])
    w2 = np.ascontiguousarray(np.asarray(mlp_w2, f32).reshape(4, NFC, 128, D))
    wq, wkv, wqi, wki, wwi, wao = [], [], [], [], [], []
    for j in range(2):
        W = np.asarray(attn_w_in[j], f32)
        wq.append(colblocks(W[:, 0:2048], 256))
        wkv.append(colblocks(W[:, 2048:3072], 256))
        wqi.append(colblocks(W[:, 3072:4096], 256))
        kicols = W[:, 4096:4160]
        wki.append(colblocks(np.concatenate([kicols, kicols], axis=1), 128)[0])
        wwi.append(colblocks(W[:, 4160:4176], 16)[0])
        wao.append(colblocks(np.asarray(attn_w_out[j], f32), 256))
    cwin = np.stack([colblocks(np.asarray(conv_w_in[j], f32), 128) for j in range(2)])
    cwout = np.stack([colblocks(np.asarray(conv_w_out[j], f32), 256) for j in range(2)])
    shared = {
        "ident": ident, "convw": convw, "tri": tri, "ropeinv": ropeinv,
        "mlp_g": np.ascontiguousarray(np.asarray(mlp_norm_g, f32)),
        "attn_g": np.ascontiguousarray(np.asarray(attn_norm_g, f32)),
        "conv_g": np.ascontiguousarray(np.asarray(conv_norm_g, f32)),
        "q_g": np.ascontiguousarray(np.asarray(attn_q_norm_g, f32)),
        "k_g": np.ascontiguousarray(np.asarray(attn_k_norm_g, f32)),
        "w1": w1, "w2": w2, "wq": np.stack(wq), "wkv": np.stack(wkv), "wqi": np.stack(wqi),
        "wki": np.stack(wki), "wwi": np.stack(wwi), "wao": np.stack(wao), "cwin": cwin, "cwout": cwout,
    }
    in_maps = []
    for c in range(8):
        b, hf = c // 2, c % 2
        p_oth = positions[b, 0:TOK].reshape(16, 128).T
        p_own = positions[b, hf * TOK:(hf + 1) * TOK].reshape(16, 128).T
        m = dict(shared)
        m["xown"] = np.ascontiguousarray(xc[c])
        m["xoth"] = np.ascontiguousarray(xc[2 * b])
        m["pos"] = np.ascontiguousarray(np.concatenate([p_oth, p_own], axis=1).astype(np.int32))
        m["halo_m"] = np.full((128, 1), float(hf), f32)
        in_maps.append(m)
    if not _PROG:
        _PROG.append(build_fused())
    res = run_bass_kernel_spmd(_PROG[0], in_maps, core_ids=list(range(8)))
    out = np.stack([np.asarray(res.results[c]["y"], f32) for c in range(8)], 0).reshape(4, 4096, D)
    return out
```
